# Optimizing a Trainium2 kernel written in Bass

```python
import math
import jax
import jax.numpy as jnp
from jax import lax
import numpy as np

D_MODEL = 1024
BATCH = 16
SEQ = 2048
DEPTH = 2

NORM_EPS = 1e-5
N_MOD = 6

HEAD_DIM = 64
N_Q_HEADS = 16
N_KV_HEADS = 4
Q_PER_KV = N_Q_HEADS // N_KV_HEADS
ATTN_WIDTH = N_Q_HEADS * HEAD_DIM
KV_WIDTH = N_KV_HEADS * HEAD_DIM
WINDOW = 128
ATTN_BLOCK = 128
ROPE_THETA = 500000.0
ROPE_DIMS = HEAD_DIM // 4
NEG_BIG = -1e30

SSM_INNER = 2 * D_MODEL
SSM_HEAD_DIM = 64
SSM_HEADS = SSM_INNER // SSM_HEAD_DIM
SSM_GROUPS = 4
SSM_HEADS_PER_GROUP = SSM_HEADS // SSM_GROUPS
SSM_STATE = 128
SSM_CONV = 5
SSM_CHUNK = 128
SSM_XBC = SSM_INNER + 2 * SSM_GROUPS * SSM_STATE

N_BRANCH = 2
N_EXPERTS = 32
TOP_K = 4
EXPERT_FF = D_MODEL
SWIGLU_LIMIT = 7.0
SWIGLU_ALPHA = 1.702

IN_SIZES = (ATTN_WIDTH, KV_WIDTH, KV_WIDTH, SSM_INNER, SSM_XBC, 2 * SSM_HEADS, N_BRANCH * D_MODEL)
IN_TOTAL = sum(IN_SIZES)
IN_OFFSETS = tuple(int(v) for v in np.cumsum(IN_SIZES)[:-1])

kernel_name = "hybrid_swa_ssd_moe_encoder"


def rms_norm(x, g):
    xf = x.astype(jnp.float32)
    y = xf * lax.rsqrt(jnp.mean(xf * xf, axis=-1, keepdims=True) + NORM_EPS)
    return (y * g.astype(jnp.float32)).astype(x.dtype)


def rope_tables(positions):
    inv_freq = ROPE_THETA ** (-jnp.arange(0, ROPE_DIMS, 2, dtype=jnp.float32) / ROPE_DIMS)
    ang = positions.astype(jnp.float32)[..., None] * inv_freq
    return jnp.cos(ang)[:, :, None, :], jnp.sin(ang)[:, :, None, :]


def apply_partial_rope(t, cos, sin):
    half = ROPE_DIMS // 2
    tf = t.astype(jnp.float32)
    t1, t2, rest = tf[..., :half], tf[..., half:ROPE_DIMS], tf[..., ROPE_DIMS:]
    return jnp.concatenate([t1 * cos - t2 * sin, t2 * cos + t1 * sin, rest], axis=-1).astype(t.dtype)


def windowed_gqa_with_sink(q, k, v, sink):
    bsz, s = q.shape[0], q.shape[1]
    nb = s // ATTN_BLOCK
    qb = q.reshape(bsz, nb, ATTN_BLOCK, N_KV_HEADS, Q_PER_KV, HEAD_DIM)
    pad = ((0, 0), (ATTN_BLOCK, ATTN_BLOCK), (0, 0), (0, 0))
    kp = jnp.pad(k, pad).reshape(bsz, nb + 2, ATTN_BLOCK, N_KV_HEADS, HEAD_DIM)
    vp = jnp.pad(v, pad).reshape(bsz, nb + 2, ATTN_BLOCK, N_KV_HEADS, HEAD_DIM)
    kb = jnp.concatenate([kp[:, :-2], kp[:, 1:-1], kp[:, 2:]], axis=2)
    vb = jnp.concatenate([vp[:, :-2], vp[:, 1:-1], vp[:, 2:]], axis=2)
    scores = jnp.einsum('bnqkrd,bnskd->bnkrqs', qb, kb).astype(jnp.float32) * (HEAD_DIM ** -0.5)
    blk = jnp.arange(nb)[:, None, None] * ATTN_BLOCK
    q_idx = blk + jnp.arange(ATTN_BLOCK)[None, :, None]
    k_idx = blk - ATTN_BLOCK + jnp.arange(3 * ATTN_BLOCK)[None, None, :]
    valid = (jnp.abs(q_idx - k_idx) <= WINDOW) & (k_idx >= 0) & (k_idx < s)
    scores = jnp.where(valid[None, :, None, None], scores, NEG_BIG)
    sink_b = sink.astype(jnp.float32).reshape(N_KV_HEADS, Q_PER_KV)[None, None, :, :, None, None]
    m = jnp.maximum(jnp.max(scores, axis=-1, keepdims=True), sink_b)
    p = jnp.exp(scores - m)
    p = p / (jnp.sum(p, axis=-1, keepdims=True) + jnp.exp(sink_b - m))
    out = jnp.einsum('bnkrqs,bnskd->bnqkrd', p.astype(v.dtype), vb)
    return out.reshape(bsz, s, ATTN_WIDTH)


def centred_depthwise_conv(u, w, b):
    out = lax.conv_general_dilated(
        u, w[:, None, :].astype(u.dtype), window_strides=(1,),
        padding=[(SSM_CONV // 2, SSM_CONV // 2)],
        dimension_numbers=('NWC', 'WIO', 'NWC'), feature_group_count=u.shape[-1])
    return out + b.astype(u.dtype)


def ssd_scan(xs, dt, a, bs, cs):
    bsz, s = xs.shape[0], xs.shape[1]
    nc = s // SSM_CHUNK
    q, g, r = SSM_CHUNK, SSM_GROUPS, SSM_HEADS_PER_GROUP
    xdt = (xs * dt[..., None]).reshape(bsz, nc, q, g, r, SSM_HEAD_DIM)
    da = (dt * a).reshape(bsz, nc, q, g, r).transpose(0, 3, 4, 1, 2)
    bc = bs.reshape(bsz, nc, q, g, SSM_STATE)
    cc = cs.reshape(bsz, nc, q, g, SSM_STATE)
    a_cum = jnp.cumsum(da, axis=-1)
    t = jnp.arange(q)
    lower = t[:, None] >= t[None, :]
    decay_in = jnp.exp(jnp.where(lower, a_cum[..., :, None] - a_cum[..., None, :], -jnp.inf))
    cb = jnp.einsum('bclgn,bcsgn->bgcls', cc, bc)
    y_diag = jnp.einsum('bgrcls,bcsgrp->bclgrp', cb[:, :, None] * decay_in, xdt)
    decay_to_end = jnp.exp(a_cum[..., -1:] - a_cum)
    states = jnp.einsum('bclgn,bgrcl,bclgrp->bcgrpn', bc, decay_to_end, xdt)
    chunk_sum = jnp.pad(a_cum[..., -1], ((0, 0), (0, 0), (0, 0), (1, 0)))
    ccum = jnp.cumsum(chunk_sum, axis=-1)
    zi = jnp.arange(nc + 1)
    decay_chunk = jnp.exp(jnp.where(zi[:, None] >= zi[None, :], ccum[..., :, None] - ccum[..., None, :], -jnp.inf))
    states = jnp.concatenate([jnp.zeros_like(states[:, :1]), states], axis=1)
    states_in = jnp.einsum('bgrzc,bcgrpn->bzgrpn', decay_chunk, states)[:, :-1]
    y_off = jnp.einsum('bclgn,bcgrpn,bgrcl->bclgrp', cc, states_in, jnp.exp(a_cum))
    return (y_diag + y_off).reshape(bsz, s, SSM_HEADS, SSM_HEAD_DIM)


def _flip(u):
    return jnp.flip(u, axis=1)


def bidirectional_ssd_mixer(z, xbc, dt_raw, conv_w, conv_b, a_log, dt_bias, d_skip, norm_g):
    bsz, s = z.shape[0], z.shape[1]
    xbc = jax.nn.silu(centred_depthwise_conv(xbc, conv_w, conv_b)).astype(jnp.float32)
    xs, bs, cs = jnp.split(xbc, [SSM_INNER, SSM_INNER + SSM_GROUPS * SSM_STATE], axis=-1)
    xs = xs.reshape(bsz, s, SSM_HEADS, SSM_HEAD_DIM)
    bs = bs.reshape(bsz, s, SSM_GROUPS, SSM_STATE)
    cs = cs.reshape(bsz, s, SSM_GROUPS, SSM_STATE)
    dt = jax.nn.softplus(dt_raw.astype(jnp.float32).reshape(bsz, s, 2, SSM_HEADS) + dt_bias.astype(jnp.float32))
    a = -jnp.exp(a_log.astype(jnp.float32))
    y = (ssd_scan(xs, dt[:, :, 0], a[0], bs, cs)
         + _flip(ssd_scan(_flip(xs), _flip(dt[:, :, 1]), a[1], _flip(bs), _flip(cs)))
         + xs * d_skip.astype(jnp.float32)[:, None])
    y = y.reshape(bsz, s, SSM_INNER) * jax.nn.silu(z.astype(jnp.float32))
    y = y.reshape(bsz, s, SSM_GROUPS, SSM_INNER // SSM_GROUPS)
    y = y * lax.rsqrt(jnp.mean(y * y, axis=-1, keepdims=True) + NORM_EPS)
    return (y.reshape(bsz, s, SSM_INNER) * norm_g.astype(jnp.float32)).astype(z.dtype)


def hybrid_mixer(h, cos, sin, w_in, q_norm_g, k_norm_g, attn_sink, conv_w, conv_b,
                 a_log, dt_bias, ssm_d, ssm_norm_g, w_attn_o, w_ssm_o, w_out):
    bsz, s = h.shape[0], h.shape[1]
    proj = jnp.einsum('bsd,de->bse', h, w_in)
    q, k, v, z, xbc, dt_raw, gates = jnp.split(proj, IN_OFFSETS, axis=-1)
    q = apply_partial_rope(rms_norm(q.reshape(bsz, s, N_Q_HEADS, HEAD_DIM), q_norm_g), cos, sin)
    k = apply_partial_rope(rms_norm(k.reshape(bsz, s, N_KV_HEADS, HEAD_DIM), k_norm_g), cos, sin)
    v = v.reshape(bsz, s, N_KV_HEADS, HEAD_DIM)
    y_attn = jnp.einsum('bse,ed->bsd', windowed_gqa_with_sink(q, k, v, attn_sink), w_attn_o)
    y_ssm = jnp.einsum('bse,ed->bsd',
                       bidirectional_ssd_mixer(z, xbc, dt_raw, conv_w, conv_b, a_log, dt_bias, ssm_d, ssm_norm_g),
                       w_ssm_o)
    g_attn, g_ssm = jnp.split(jax.nn.sigmoid(gates), N_BRANCH, axis=-1)
    return jnp.einsum('bsd,de->bse', g_attn * y_attn + g_ssm * y_ssm, w_out)


def moe_clamped_swiglu(h, router_w, router_b, w_gate, b_gate, w_up, b_up, w_down, b_down):
    bsz, s, d = h.shape
    tok = h.reshape(bsz * s, d)
    logits = (tok @ router_w + router_b).astype(jnp.float32)
    top_v, top_i = lax.top_k(logits, TOP_K)
    top_w = jax.nn.softmax(top_v, axis=-1)
    combine = jnp.sum(jax.nn.one_hot(top_i, N_EXPERTS, dtype=jnp.float32) * top_w[..., None], axis=1).astype(h.dtype)
    out = jnp.zeros_like(tok)
    for e in range(N_EXPERTS):
        glu = jnp.minimum(tok @ w_gate[e] + b_gate[e], SWIGLU_LIMIT)
        lin = jnp.clip(tok @ w_up[e] + b_up[e], -SWIGLU_LIMIT, SWIGLU_LIMIT)
        act = glu * jax.nn.sigmoid(SWIGLU_ALPHA * glu) * (lin + 1.0)
        out = out + combine[:, e:e + 1] * (act @ w_down[e] + b_down[e])
    return out.reshape(bsz, s, d)


def setup_inputs(seed: int = 0) -> dict:
    key = jax.random.key(seed)
    ks = jax.random.split(key, 28)
    f32 = jnp.float32
    L = DEPTH

    def nrm(k, shape, scale):
        return jax.random.normal(k, shape, f32) * scale

    offsets = jax.random.randint(ks[2], (BATCH, 1), 0, 4096, dtype=jnp.int32)
    positions = jnp.arange(SEQ, dtype=jnp.int32)[None, :] + offsets
    dt0 = jnp.exp(jax.random.uniform(ks[13], (L, 2, SSM_HEADS), f32, math.log(1e-3), math.log(1e-1)))
    return {
        'x': nrm(ks[0], (BATCH, SEQ, D_MODEL), 1.0),
        'c': nrm(ks[1], (BATCH, D_MODEL), 1.0),
        'positions': positions,
        'ada_w': nrm(ks[3], (L, D_MODEL, N_MOD * D_MODEL), 0.5 * D_MODEL ** -0.5),
        'ada_b': nrm(ks[4], (L, N_MOD * D_MODEL), 0.02),
        'norm1_g': 1.0 + nrm(ks[5], (L, D_MODEL), 0.02),
        'norm2_g': 1.0 + nrm(ks[6], (L, D_MODEL), 0.02),
        'w_in': nrm(ks[7], (L, D_MODEL, IN_TOTAL), D_MODEL ** -0.5),
        'q_norm_g': 1.0 + nrm(ks[8], (L, HEAD_DIM), 0.02),
        'k_norm_g': 1.0 + nrm(ks[9], (L, HEAD_DIM), 0.02),
        'attn_sink': nrm(ks[10], (L, N_Q_HEADS), 0.5),
        'conv_w': nrm(ks[11], (L, SSM_CONV, SSM_XBC), SSM_CONV ** -0.5),
        'conv_b': nrm(ks[12], (L, SSM_XBC), 0.02),
        'a_log': jnp.log(jax.random.uniform(ks[14], (L, 2, SSM_HEADS), f32, 1.0, 16.0)),
        'dt_bias': dt0 + jnp.log(-jnp.expm1(-dt0)),
        'ssm_d': 1.0 + nrm(ks[15], (L, SSM_HEADS), 0.1),
        'ssm_norm_g': 1.0 + nrm(ks[16], (L, SSM_INNER), 0.02),
        'w_attn_o': nrm(ks[17], (L, ATTN_WIDTH, D_MODEL), ATTN_WIDTH ** -0.5),
        'w_ssm_o': nrm(ks[18], (L, SSM_INNER, D_MODEL), SSM_INNER ** -0.5),
        'w_out': nrm(ks[19], (L, D_MODEL, D_MODEL), D_MODEL ** -0.5),
        'router_w': nrm(ks[20], (L, D_MODEL, N_EXPERTS), D_MODEL ** -0.5),
        'router_b': nrm(ks[21], (L, N_EXPERTS), 0.01),
        'exp_w_gate': nrm(ks[22], (L, N_EXPERTS, D_MODEL, EXPERT_FF), D_MODEL ** -0.5),
        'exp_b_gate': nrm(ks[23], (L, N_EXPERTS, EXPERT_FF), 0.01),
        'exp_w_up': nrm(ks[24], (L, N_EXPERTS, D_MODEL, EXPERT_FF), D_MODEL ** -0.5),
        'exp_b_up': nrm(ks[25], (L, N_EXPERTS, EXPERT_FF), 0.01),
        'exp_w_down': nrm(ks[26], (L, N_EXPERTS, EXPERT_FF, D_MODEL), EXPERT_FF ** -0.5),
        'exp_b_down': nrm(ks[27], (L, N_EXPERTS, D_MODEL), 0.01),
    }


def reference(x, c, positions, ada_w, ada_b, norm1_g, norm2_g, w_in, q_norm_g, k_norm_g,
              attn_sink, conv_w, conv_b, a_log, dt_bias, ssm_d, ssm_norm_g, w_attn_o,
              w_ssm_o, w_out, router_w, router_b, exp_w_gate, exp_b_gate, exp_w_up,
              exp_b_up, exp_w_down, exp_b_down):
    cos, sin = rope_tables(positions)
    c_act = jax.nn.silu(c)
    for l in range(DEPTH):
        mod = jnp.einsum('bd,de->be', c_act, ada_w[l]) + ada_b[l]
        sh1, sc1, g1, sh2, sc2, g2 = jnp.split(mod[:, None, :], N_MOD, axis=-1)
        h = rms_norm(x, norm1_g[l]) * (1.0 + sc1) + sh1
        x = x + g1 * hybrid_mixer(h, cos, sin, w_in[l], q_norm_g[l], k_norm_g[l], attn_sink[l],
                                  conv_w[l], conv_b[l], a_log[l], dt_bias[l], ssm_d[l],
                                  ssm_norm_g[l], w_attn_o[l], w_ssm_o[l], w_out[l])
        h = rms_norm(x, norm2_g[l]) * (1.0 + sc2) + sh2
        x = x + g2 * moe_clamped_swiglu(h, router_w[l], router_b[l], exp_w_gate[l], exp_b_gate[l],
                                        exp_w_up[l], exp_b_up[l], exp_w_down[l], exp_b_down[l])
    return x
```

```python
import contextlib
import numpy as np
import concourse.bass as bass
import concourse.mybir as mybir
from concourse.bass_utils import run_bass_kernel_spmd

F32 = mybir.dt.float32
BF16 = mybir.dt.bfloat16
I32 = mybir.dt.int32
AF = mybir.ActivationFunctionType
ALU = mybir.AluOpType
AX = mybir.AxisListType

D = 1024
L = 2
NE = 32
EPS = 1e-5
IN_TOTAL = 8768
OQ, OK_, OV, OZ, OXBC, ODT, OG = 0, 1024, 1280, 1536, 3584, 6656, 6720
NTM = 3648
SAME_ENG_SYNC = True
XP = 2


class Buf:
    def __init__(self, t, name=""):
        self.t = t
        self.name = name
        self.writers = []
        self.readers = []
        self.gen_open = False
        self.gen_deps = []

    def __getitem__(self, k):
        return self.t[k]


class Q:
    def __init__(self, kk, eng, name, ndma=0):
        self.eng = eng
        self.name = name
        self.sem = kk.newsem(name)
        self.cnt = 0
        self.waited = {}
        self.dma = [[kk.newsem("%s_d%d" % (name, i)), 0] for i in range(ndma)]
        self.dma_i = 0


class K:
    def __init__(self, nc, es):
        self.nc = nc
        self.es = es
        self.sems = []
        self.pe = Q(self, nc.tensor, "pe")
        self.act = Q(self, nc.scalar, "act", 8)
        self.dve = Q(self, nc.vector, "dve")
        self.pool = Q(self, nc.gpsimd, "pool", 24)
        self.sp = Q(self, nc.sync, "sp", 24)
        self.qs = [self.pe, self.act, self.dve, self.pool, self.sp]
        self.ninstr = 0

    def newsem(self, name):
        s = self.es.enter_context(self.nc.semaphore(name))
        self.sems.append(s)
        return len(self.sems) - 1

    def wait(self, q, tok):
        si, val = tok
        if val <= 0:
            return
        if q.waited.get(si, 0) >= val:
            return
        q.eng.wait_ge(self.sems[si], val)
        q.waited[si] = val
        self.ninstr += 1

    def _deps(self, q, r, w, j):
        deps = []
        for b in r:
            deps += b.writers
        for b in w:
            d = b.writers + b.readers
            deps += d
        for b in j:
            if b.gen_open:
                deps += b.gen_deps
            else:
                d = b.writers + b.readers
                b.gen_deps = d
                deps += d
        for t in set(deps):
            if (not SAME_ENG_SYNC) and t[0] == q.sem:
                continue
            self.wait(q, t)

    def _post(self, tok, r, w, j):
        for b in r:
            b.readers.append(tok)
            b.gen_open = False
        for b in w:
            b.writers = [tok]
            b.readers = []
            b.gen_open = False
        for b in j:
            if b.gen_open:
                b.writers.append(tok)
            else:
                b.writers = [tok]
                b.readers = []
                b.gen_open = True

    def op(self, q, fn, r=(), w=(), j=()):
        self._deps(q, r, w, j)
        ins = fn()
        q.cnt += 1
        ins.then_inc(self.sems[q.sem], 1)
        self.ninstr += 1
        self._post((q.sem, q.cnt), r, w, j)

    def dma(self, q, out, in_, r=(), w=(), j=(), **kw):
        self._deps(q, r, w, j)
        slot = q.dma[q.dma_i % len(q.dma)]
        q.dma_i += 1
        self.wait(q, (slot[0], slot[1]))
        ins = q.eng.dma_start(out=out, in_=in_, **kw)
        slot[1] += 16
        ins.then_inc(self.sems[slot[0]], 16)
        self.ninstr += 1
        self._post((slot[0], slot[1]), r, w, j)

    def barrier(self):
        toks = [(q.sem, q.cnt) for q in self.qs]
        for q in self.qs:
            for s in q.dma:
                toks.append((s[0], s[1]))
        for q in self.qs:
            for t in toks:
                if t[0] == q.sem:
                    continue
                self.wait(q, t)


def build(NB, S, nlayers=L, debug=False, stop=None, ne_decl=NE):
    T = NB * S
    NT = T // 128
    NTS = S // 128
    TG = 512 if S % 512 == 0 else S
    NTG = S // TG
    nc = bass.Bass("TRN2", target_bir_lowering=False)
    dbgkind = "ExternalOutput" if debug else "Internal"

    def din(name, shape, dt=F32):
        return nc.dram_tensor(name, list(shape), dt, kind="ExternalInput").ap()

    def dscr(name, shape, dt=F32):
        return nc.dram_tensor(name, list(shape), dt, kind=dbgkind).ap()

    x_in = din("x", [T, D])
    cT_in = din("cT", [128, 8, NB])
    pos_in = din("pos", [128, NT], I32)
    invf_in = din("invf", [128, 8])
    ada_w = din("ada_w", [L, D, 6 * D])
    ada_b = din("ada_b", [L, 6 * D])
    norm1_g = din("norm1_g", [L, D])
    norm2_g = din("norm2_g", [L, D])
    w_in = din("w_in", [L, D, IN_TOTAL])
    q_norm_g = din("q_norm_g", [L, 64])
    k_norm_g = din("k_norm_g", [L, 64])
    attn_sink = din("attn_sink", [L, 16])
    conv_w = din("conv_w", [L, 128, 24, 5])
    conv_b = din("conv_b", [L, 128, 24])
    a_log = din("a_log", [L, 64])
    dt_bias = din("dt_bias", [L, 64])
    ssm_d = din("ssm_d", [L, 32])
    ssm_norm_g = din("ssm_norm_g", [L, 2048])
    w_attn_o = din("w_attn_o", [L, D, D])
    w_ssm_o = din("w_ssm_o", [L, 2 * D, D])
    w_out = din("w_out", [L, D, D])
    router_w = din("router_w", [L, D, NE])
    router_b = din("router_b", [L, NE])
    w_gate = din("exp_w_gate", [L, ne_decl, D, D])
    b_gate = din("exp_b_gate", [L, 128, NE, 8])
    w_up = din("exp_w_up", [L, ne_decl, D, D])
    b_up = din("exp_b_up", [L, 128, NE, 8])
    w_down = din("exp_w_down", [L, ne_decl, D, D])
    b_down = din("exp_b_down", [L, NE, D])
    y_out = nc.dram_tensor("y", [T, D], F32, kind="ExternalOutput").ap()

    xres = dscr("xres", [T, D])
    modd = dscr("modd", [L, NB, 6 * D])
    proj_tm = dscr("proj_tm", [T, NTM])
    xs_tm = dscr("xs_tm", [T, 2048])
    bt_d = dscr("bt_d", [4, 128, T], BF16)
    ct_d = dscr("ct_d", [4, 128, T], BF16)
    btm_d = dscr("btm_d", [T, 512], BF16)
    gT_d = dscr("gT_d", [2048, T])
    gst_d = dscr("gst_d", [NB, NTS, 128, 2048], BF16)

    dbg_aoT = dscr("dbg_aoT", [128, 8, S], BF16)
    dbg_ysT = dscr("dbg_ysT", [128, 16, S], BF16)
    dbg_comb = dscr("dbg_comb", [128, S // 128, NE])
    es = contextlib.ExitStack()
    with es:
        kk = K(nc, es)
        pe, act, dve, pool, sp = kk.pe, kk.act, kk.dve, kk.pool, kk.sp
        V = nc.vector
        A = nc.scalar
        G = nc.gpsimd
        PE = nc.tensor

        uid = [0]

        def sb(st, name, shape, dt=F32):
            uid[0] += 1
            name = "s%d_%s" % (uid[0], name)
            return Buf(st.enter_context(nc.sbuf_tensor(name, list(shape), dt)), name)

        def ps(st, name, shape, dt=F32):
            uid[0] += 1
            name = "p%d_%s" % (uid[0], name)
            esz = 2 if dt == BF16 else 4
            nel = 1
            for d_ in shape[1:]:
                nel *= d_
            assert nel * esz <= 2048
            t = st.enter_context(nc.psum_tensor(name, [128, 2048 // esz], dt))
            v = t[0:shape[0], 0:nel]
            if len(shape) == 3:
                v = v.rearrange("p (a b) -> p a b", b=shape[2])
            return Buf(v, name)

        identf = sb(es, "identf", [128, 128])
        identb = sb(es, "identb", [128, 128], BF16)
        ones_f = sb(es, "ones_f", [128, 128])
        triF = sb(es, "triF", [128, 128])
        triB = sb(es, "triB", [128, 128])
        sgtF = sb(es, "sgtF", [128, 128])
        sltB = sb(es, "sltB", [128, 128])
        cosT = sb(es, "cosT", [128, NT, 8])
        sinT = sb(es, "sinT", [128, NT, 8])

        def aff(buf, pattern_step, cmul, base, cmp):
            kk.op(pool, lambda: G.memset(buf[:], 1.0), w=[buf])
            kk.op(pool, lambda: G.affine_select(out=buf[:], in_=buf[:], pattern=[[pattern_step, 128]],
                                                compare_op=cmp, fill=0.0, base=base, channel_multiplier=cmul),
                  r=[buf], w=[buf])

        aff(identf, -1, 1, 0, ALU.is_equal)
        aff(triF, 1, -1, 0, ALU.is_ge)
        aff(triB, -1, 1, 0, ALU.is_ge)
        aff(sgtF, -1, 1, 0, ALU.is_gt)
        aff(sltB, 1, -1, 0, ALU.is_gt)
        kk.op(pool, lambda: G.memset(ones_f[:], 1.0), w=[ones_f])
        kk.op(dve, lambda: V.tensor_copy(out=identb[:], in_=identf[:]), r=[identf], w=[identb])

        with contextlib.ExitStack() as st:
            posi = sb(st, "posi", [128, NT], I32)
            posf = sb(st, "posf", [128, NT])
            invf = sb(st, "invf", [128, 8])
            ang = sb(st, "ang", [128, NT, 8])
            kf = sb(st, "kf", [128, NT, 8])
            ki = sb(st, "ki", [128, NT, 8], I32)
            rr = sb(st, "rr", [128, NT, 8])
            t2 = sb(st, "rt2", [128, NT, 8])
            kk.dma(sp, posi[:], pos_in[:, :], w=[posi])
            kk.dma(sp, invf[:], invf_in[:, :], w=[invf])
            kk.op(dve, lambda: V.tensor_copy(out=posf[:], in_=posi[:]), r=[posi], w=[posf])
            kk.op(dve, lambda: V.tensor_tensor(out=ang[:], in0=posf[:].unsqueeze(2).to_broadcast([128, NT, 8]),
                                               in1=invf[:].unsqueeze(1).to_broadcast([128, NT, 8]), op=ALU.mult),
                  r=[posf, invf], w=[ang])
            TWO_PI = float(2 * np.pi)
            C1 = 6.28125
            C2 = float(2 * np.pi - 6.28125)
            for (dst, shift) in ((sinT, 0.0), (cosT, float(np.pi / 2))):
                kk.op(dve, lambda: V.tensor_scalar(out=kf[:], in0=ang[:], scalar1=shift, scalar2=1.0 / TWO_PI,
                                                   op0=ALU.add, op1=ALU.mult), r=[ang], w=[kf])
                kk.op(dve, lambda: V.tensor_copy(out=ki[:], in_=kf[:]), r=[kf], w=[ki])
                kk.op(dve, lambda: V.tensor_copy(out=kf[:], in_=ki[:]), r=[ki], w=[kf])
                kk.op(dve, lambda: V.tensor_scalar(out=rr[:], in0=ang[:], scalar1=shift, scalar2=None, op0=ALU.add),
                      r=[ang], w=[rr])
                kk.op(dve, lambda: V.scalar_tensor_tensor(out=rr[:], in0=kf[:], scalar=-C1, in1=rr[:],
                                                          op0=ALU.mult, op1=ALU.add), r=[kf, rr], w=[rr])
                kk.op(dve, lambda: V.scalar_tensor_tensor(out=rr[:], in0=kf[:], scalar=-C2, in1=rr[:],
                                                          op0=ALU.mult, op1=ALU.add), r=[kf, rr], w=[rr])
                kk.op(dve, lambda: V.tensor_scalar(out=t2[:], in0=rr[:], scalar1=float(np.pi), scalar2=-TWO_PI,
                                                   op0=ALU.is_gt, op1=ALU.mult), r=[rr], w=[t2])
                kk.op(dve, lambda: V.tensor_tensor(out=rr[:], in0=rr[:], in1=t2[:], op=ALU.add), r=[rr, t2], w=[rr])
                kk.op(dve, lambda: V.tensor_scalar(out=t2[:], in0=rr[:], scalar1=float(-np.pi), scalar2=TWO_PI,
                                                   op0=ALU.is_lt, op1=ALU.mult), r=[rr], w=[t2])
                kk.op(dve, lambda: V.tensor_tensor(out=rr[:], in0=rr[:], in1=t2[:], op=ALU.add), r=[rr, t2], w=[rr])
                kk.op(dve, lambda: V.tensor_scalar(out=rr[:], in0=rr[:], scalar1=float(np.pi), scalar2=float(-np.pi),
                                                   op0=ALU.min, op1=ALU.max), r=[rr], w=[rr])
                kk.op(act, lambda: A.activation(out=dst[:], in_=rr[:], func=AF.Sin), r=[rr], w=[dst])
            kk.barrier()

        kk.dma(sp, xres[:, :], x_in[:, :])
        kk.barrier()

        def bc_load(q, buf, src_row_ap):
            kk.dma(q, buf[:], src_row_ap.partition_broadcast(128), w=[buf])

        def phase_mod(l):
            with contextlib.ExitStack() as st:
                cact = sb(st, "cact", [128, 8, NB])
                adab = sb(st, "adab", [1, 6 * D])
                modsb = sb(st, "modsb", [1, NB, 6 * D])
                wch = [sb(st, "adaw%d" % i, [128, 8, 512]) for i in range(2)]
                pm = [ps(st, "pmod%d" % i, [1, 512]) for i in range(2)]
                kk.dma(sp, cact[:], cT_in[:, :, :], w=[cact])
                kk.dma(sp, adab[:], ada_b[l:l + 1, :], w=[adab])
                kk.op(act, lambda: A.activation(out=cact[:], in_=cact[:], func=AF.Silu), r=[cact], w=[cact])
                for n in range(12):
                    wb = wch[n % 2]
                    kk.dma(sp, wb[:], ada_w[l, :, n * 512:(n + 1) * 512].rearrange("(k p) n -> p k n", p=128), w=[wb])
                    for b in range(NB):
                        pp = pm[(n * NB + b) % 2]
                        for k in range(8):
                            kk.op(pe, lambda: PE.matmul(pp[:], lhsT=cact[:, k, b:b + 1], rhs=wb[:, k, :],
                                                        start=(k == 0), stop=(k == 7)),
                                  r=[cact, wb], j=[pp])
                        kk.op(dve, lambda: V.tensor_tensor(out=modsb[0:1, b, n * 512:(n + 1) * 512], in0=pp[:],
                                                           in1=adab[0:1, n * 512:(n + 1) * 512], op=ALU.add),
                              r=[pp, adab], j=[modsb])
                for b in range(NB):
                    for idx in (1, 4):
                        kk.op(dve, lambda: V.tensor_scalar(out=modsb[0:1, b, idx * D:(idx + 1) * D],
                                                           in0=modsb[0:1, b, idx * D:(idx + 1) * D],
                                                           scalar1=1.0, scalar2=None, op0=ALU.add),
                              r=[modsb], w=[modsb])
                kk.dma(sp, modd[l:l + 1, :, :], modsb[:], r=[modsb])
                kk.barrier()

        def norm_mod_tile(st_bufs, i, l, gidx, hT, src):
            (xt, sq, ss, hb, G_bc, SH_bc, ptr) = st_bufs
            sq = sq[i % 2]
            ss = ss[i % 2]
            b = i // NTS
            x_t = xt[i % 2]
            h_b = hb[i % 2]
            p_t = ptr[i % 2]
            kk.dma(sp, x_t[:], src[i * 128:(i + 1) * 128, :], w=[x_t])
            kk.op(act, lambda: A.activation(out=sq[:], in_=x_t[:], func=AF.Square, accum_out=ss[:, 0:1]),
                  r=[x_t], w=[sq, ss])
            kk.op(act, lambda: A.activation(out=ss[:, 1:2], in_=ss[:, 0:1], func=AF.Sqrt, scale=1.0 / D, bias=epsb[:, 0:1]),
                  r=[ss, epsb], w=[ss])
            kk.op(dve, lambda: V.reciprocal(out=ss[:, 2:3], in_=ss[:, 1:2]), r=[ss], w=[ss])
            kk.op(dve, lambda: V.scalar_tensor_tensor(out=sq[:], in0=x_t[:], scalar=ss[:, 2:3], in1=G_bc[b][:],
                                                      op0=ALU.mult, op1=ALU.mult), r=[x_t, ss, G_bc[b]], w=[sq])
            kk.op(pool, lambda: G.tensor_tensor(out=h_b[:], in0=sq[:], in1=SH_bc[b][:], op=ALU.add),
                  r=[sq, SH_bc[b]], w=[h_b])
            for k in range(8):
                kk.op(pe, lambda: PE.transpose(out=p_t[:, k, :], in_=h_b[:, k * 128:(k + 1) * 128], identity=identb[:]),
                      r=[h_b, identb], j=[p_t])
            kk.op(act, lambda: A.copy(out=hT[:, :, i * 128:(i + 1) * 128], in_=p_t[:]), r=[p_t], j=[hT])
            return x_t, sq, h_b

        epsb = sb(es, "epsb", [128, 1])
        kk.op(pool, lambda: G.memset(epsb[:], EPS), w=[epsb])

        def load_mod_bc(st, l, gidx_scale, gidx_shift, norm_g, tag):
            Gs, SHs = [], []
            ng = sb(st, "ng" + tag, [128, D])
            bc_load(sp, ng, norm_g[l:l + 1, :])
            for b in range(NB):
                g_ = sb(st, "G%s%d" % (tag, b), [128, D])
                s_ = sb(st, "SH%s%d" % (tag, b), [128, D])
                bc_load(sp, g_, modd[l, b:b + 1, gidx_scale * D:(gidx_scale + 1) * D])
                bc_load(sp, s_, modd[l, b:b + 1, gidx_shift * D:(gidx_shift + 1) * D])
                kk.op(dve, lambda: V.tensor_tensor(out=g_[:], in0=g_[:], in1=ng[:], op=ALU.mult), r=[g_, ng], w=[g_])
                Gs.append(g_)
                SHs.append(s_)
            return Gs, SHs

        def phase_inproj(l):
            with contextlib.ExitStack() as st:
                hT = sb(st, "hT", [128, 8, T], BF16)
                with contextlib.ExitStack() as st1:
                    Gs, SHs = load_mod_bc(st1, l, 1, 0, norm1_g, "a")
                    xt = [sb(st1, "xt%d" % i, [128, D]) for i in range(2)]
                    sq = [sb(st1, "sq%d" % i, [128, D]) for i in range(2)]
                    ss = [sb(st1, "ss%d" % i, [128, 4]) for i in range(2)]
                    hb = [sb(st1, "hb%d" % i, [128, D], BF16) for i in range(2)]
                    ptr = [ps(st1, "ptr%d" % i, [128, 8, 128], BF16) for i in range(2)]
                    for i in range(NT):
                        norm_mod_tile((xt, sq, ss, hb, Gs, SHs, ptr), i, l, 0, hT, xres)
                    kk.barrier()
                with contextlib.ExitStack() as st2:
                    wb = [sb(st2, "winb%d" % i, [128, 8, 512], BF16) for i in range(2)]
                    stg = [sb(st2, "stg%d" % i, [128, 512]) for i in range(3)]
                    pp = [ps(st2, "ppj%d" % i, [128, 512]) for i in range(4)]
                    groups = [(c * 512, 512, c * 512) for c in range(7)] + [(ODT, 64, 3584)]
                    cnt = 0
                    for gi, (c0, w, d0) in enumerate(groups):
                        w_b = wb[gi % 2]
                        kk.dma(pool, w_b[:, :, 0:w], w_in[l, :, c0:c0 + w].rearrange("(k p) n -> p k n", p=128), w=[w_b])
                        for i in range(NT):
                            p_ = pp[cnt % 4]
                            s_ = stg[cnt % 3]
                            for k in range(8):
                                kk.op(pe, lambda: PE.matmul(p_[:, 0:w], lhsT=hT[:, k, i * 128:(i + 1) * 128],
                                                            rhs=w_b[:, k, 0:w], start=(k == 0), stop=(k == 7)),
                                      r=[hT, w_b], j=[p_])
                            if cnt % 2 == 0:
                                kk.op(act, lambda: A.copy(out=s_[:, 0:w], in_=p_[:, 0:w]), r=[p_], w=[s_])
                            else:
                                kk.op(dve, lambda: V.tensor_copy(out=s_[:, 0:w], in_=p_[:, 0:w]), r=[p_], w=[s_])
                            kk.dma(sp, proj_tm[i * 128:(i + 1) * 128, d0:d0 + w], s_[:, 0:w], r=[s_])
                            cnt += 1
                    kk.barrier()
                with contextlib.ExitStack() as st3:
                    wb = [sb(st3, "winc%d" % i, [128, 8, 512], BF16) for i in range(2)]
                    cw = sb(st3, "cw", [128, 24, 5])
                    cb = sb(st3, "cb", [128, 24])
                    xpad = [sb(st3, "xpad%d" % i, [128, S + 4]) for i in range(2)]
                    cv = [sb(st3, "cv%d" % i, [128, S]) for i in range(2)]
                    cvb = [sb(st3, "cvb%d" % i, [128, S], BF16) for i in range(2)]
                    tst = [sb(st3, "tst%d" % i, [128, 4, 128]) for i in range(2)]
                    tstb = [sb(st3, "tstb%d" % i, [128, 4, 128], BF16) for i in range(2)]
                    gsb = [sb(st3, "gsb%d" % i, [128, TG]) for i in range(2)]
                    pp = [ps(st3, "ppf%d" % i, [128, TG]) for i in range(3)]
                    ptf = [ps(st3, "ptf%d" % i, [128, 4, 128]) for i in range(2)]
                    ptb = [ps(st3, "ptb%d" % i, [128, 4, 128], BF16) for i in range(1)]
                    kk.dma(sp, cw[:], conv_w[l, :, :, :], w=[cw])
                    kk.dma(sp, cb[:], conv_b[l, :, :], w=[cb])
                    for xp in xpad:
                        kk.op(pool, lambda: G.memset(xp[:], 0.0), w=[xp])
                    cnt = 0
                    ccnt = 0
                    tcnt = 0
                    ncg = 6
                    for cg in range(ncg):
                        c0 = OXBC + cg * 512
                        w = min(512, IN_TOTAL - c0)
                        if c0 + w > ODT and c0 < OG:
                            pass
                        w_b = wb[cg % 2]
                        kk.dma(pool, w_b[:, :, 0:w], w_in[l, :, c0:c0 + w].rearrange("(k p) n -> p k n", p=128), w=[w_b])
                        for fc in range(w // 128 if w % 128 == 0 else (w + 127) // 128):
                            f0 = c0 + fc * 128
                            if f0 >= ODT:
                                continue
                            ch = (f0 - OXBC) // 128
                            for b in range(NB):
                                xp = xpad[ccnt % 2]
                                c_v = cv[ccnt % 2]
                                for tg in range(NTG):
                                    p_ = pp[cnt % 3]
                                    cnt += 1
                                    t0 = b * S + tg * TG
                                    for k in range(8):
                                        kk.op(pe, lambda: PE.matmul(p_[:], lhsT=w_b[:, k, fc * 128:(fc + 1) * 128],
                                                                    rhs=hT[:, k, t0:t0 + TG], start=(k == 0), stop=(k == 7)),
                                              r=[hT, w_b], j=[p_])
                                    kk.op(act, lambda: A.copy(out=xp[:, 2 + tg * TG:2 + (tg + 1) * TG], in_=p_[:]),
                                          r=[p_], j=[xp])
                                kk.op(dve, lambda: V.tensor_scalar(out=c_v[:], in0=xp[:, 0:S], scalar1=cw[:, ch, 0:1],
                                                                   scalar2=cb[:, ch:ch + 1], op0=ALU.mult, op1=ALU.add),
                                      r=[xp, cw, cb], w=[c_v])
                                for kq in range(1, 5):
                                    eng, E_ = (dve, V)
                                    kk.op(eng, lambda: E_.scalar_tensor_tensor(out=c_v[:], in0=xp[:, kq:kq + S],
                                                                               scalar=cw[:, ch, kq:kq + 1], in1=c_v[:],
                                                                               op0=ALU.mult, op1=ALU.add),
                                          r=[xp, cw, c_v], w=[c_v])
                                if ch < 16:
                                    kk.op(act, lambda: A.activation(out=c_v[:], in_=c_v[:], func=AF.Silu), r=[c_v], w=[c_v])
                                    for j0 in range(0, NTS, 4):
                                        nj = min(4, NTS - j0)
                                        p_t = ptf[tcnt % 2]
                                        t_s = tst[tcnt % 2]
                                        tcnt += 1
                                        for jj in range(nj):
                                            kk.op(pe, lambda: PE.transpose(out=p_t[:, jj, :],
                                                                           in_=c_v[:, (j0 + jj) * 128:(j0 + jj + 1) * 128],
                                                                           identity=identf[:]),
                                                  r=[c_v, identf], j=[p_t])
                                        kk.op(dve, lambda: V.tensor_copy(out=t_s[:, 0:nj, :], in_=p_t[:, 0:nj, :]),
                                              r=[p_t], w=[t_s])
                                        r0 = b * S + j0 * 128
                                        kk.dma(sp, xs_tm[r0:r0 + nj * 128, ch * 128:(ch + 1) * 128].rearrange("(j p) c -> p j c", p=128),
                                               t_s[:, 0:nj, :], r=[t_s])
                                else:
                                    c_b = cvb[ccnt % 2]
                                    kk.op(act, lambda: A.activation(out=c_b[:], in_=c_v[:], func=AF.Silu), r=[c_v], w=[c_b])
                                    gq = (ch - 16) % 4
                                    dst = bt_d if ch < 20 else ct_d
                                    kk.dma(sp, dst[gq, :, b * S:(b + 1) * S], c_b[:], r=[c_b])
                                    if ch < 20:
                                        for j0 in range(0, NTS, 4):
                                            nj = min(4, NTS - j0)
                                            p_t = ptb[0]
                                            t_s = tstb[tcnt % 2]
                                            tcnt += 1
                                            for jj in range(nj):
                                                kk.op(pe, lambda: PE.transpose(out=p_t[:, jj, :],
                                                                               in_=c_b[:, (j0 + jj) * 128:(j0 + jj + 1) * 128],
                                                                               identity=identb[:]),
                                                      r=[c_b, identb], j=[p_t])
                                            kk.op(dve, lambda: V.tensor_copy(out=t_s[:, 0:nj, :], in_=p_t[:, 0:nj, :]),
                                                  r=[p_t], w=[t_s])
                                            r0 = b * S + j0 * 128
                                            kk.dma(sp, btm_d[r0:r0 + nj * 128, gq * 128:(gq + 1) * 128].rearrange("(j p) c -> p j c", p=128),
                                                   t_s[:, 0:nj, :], r=[t_s])
                                ccnt += 1
                    for cg in range(4):
                        c0 = OG + cg * 512
                        w_b = wb[cg % 2]
                        kk.dma(pool, w_b[:], w_in[l, :, c0:c0 + 512].rearrange("(k p) n -> p k n", p=128), w=[w_b])
                        for fc in range(4):
                            gch = cg * 4 + fc
                            for b in range(NB):
                                for tg in range(NTG):
                                    p_ = pp[cnt % 3]
                                    g_s = gsb[cnt % 2]
                                    cnt += 1
                                    t0 = b * S + tg * TG
                                    for k in range(8):
                                        kk.op(pe, lambda: PE.matmul(p_[:], lhsT=w_b[:, k, fc * 128:(fc + 1) * 128],
                                                                    rhs=hT[:, k, t0:t0 + TG], start=(k == 0), stop=(k == 7)),
                                              r=[hT, w_b], j=[p_])
                                    kk.op(act, lambda: A.activation(out=g_s[:], in_=p_[:], func=AF.Sigmoid), r=[p_], w=[g_s])
                                    kk.dma(sp, gT_d[gch * 128:(gch + 1) * 128, t0:t0 + TG], g_s[:], r=[g_s])
                kk.barrier()

        def phase_attn(l, b, aoT):
            with contextlib.ExitStack() as st:
                qT = sb(st, "qT", [64, 16, S], BF16)
                kT = sb(st, "kT", [64, 4, S], BF16)
                vau = sb(st, "vau", [128, NTS, 4, 65], BF16)
                gq = sb(st, "gq", [128, 64])
                gk = sb(st, "gk", [128, 64])
                esk = sb(st, "esk", [128, 16])
                bc_load(sp, gq, q_norm_g[l:l + 1, :])
                bc_load(sp, gk, k_norm_g[l:l + 1, :])
                bc_load(sp, esk, attn_sink[l:l + 1, :])
                kk.op(act, lambda: A.activation(out=esk[:], in_=esk[:], func=AF.Exp), r=[esk], w=[esk])
                kk.op(pool, lambda: G.memset(vau[:], 1.0), w=[vau])
                with contextlib.ExitStack() as st1:
                    qkv = [sb(st1, "qkv%d" % i, [128, 1536]) for i in range(2)]
                    sq = sb(st1, "sqa", [128, 1280])
                    ssn = sb(st1, "ssn", [128, 20])
                    rstd = sb(st1, "rstd", [128, 20])
                    qkn = sb(st1, "qkn", [128, 1280])
                    qkr = [sb(st1, "qkr%d" % i, [128, 1280], BF16) for i in range(2)]
                    r1 = sb(st1, "r1", [128, 20, 8])
                    r2 = sb(st1, "r2", [128, 20, 8])
                    r3 = sb(st1, "r3", [128, 20, 8])
                    r4 = sb(st1, "r4", [128, 20, 8])
                    ptq = [ps(st1, "ptq%d" % i, [64, 8, 128], BF16) for i in range(4)]
                    ptk = [ps(st1, "ptk%d" % i, [64, 4, 128], BF16) for i in range(2)]
                    for j in range(NTS):
                        r0 = b * S + j * 128
                        q_ = qkv[j % 2]
                        q_r = qkr[j % 2]
                        kk.dma(sp, q_[:], proj_tm[r0:r0 + 128, 0:1536], w=[q_])
                        kk.op(dve, lambda: V.tensor_tensor(out=sq[:], in0=q_[:, 0:1280], in1=q_[:, 0:1280], op=ALU.mult),
                              r=[q_], w=[sq])
                        kk.op(dve, lambda: V.tensor_reduce(out=ssn[:], in_=sq[:].rearrange("p (h d) -> p h d", d=64),
                                                           axis=AX.X, op=ALU.add), r=[sq], w=[ssn])
                        kk.op(act, lambda: A.activation(out=rstd[:], in_=ssn[:], func=AF.Sqrt, scale=1.0 / 64, bias=epsb[:, 0:1]),
                              r=[ssn, epsb], w=[rstd])
                        kk.op(dve, lambda: V.reciprocal(out=rstd[:], in_=rstd[:]), r=[rstd], w=[rstd])
                        kk.op(dve, lambda: V.tensor_scalar(out=rstd[:, 0:16], in0=rstd[:, 0:16], scalar1=0.125, scalar2=None,
                                                           op0=ALU.mult), r=[rstd], w=[rstd])
                        q3 = q_[:, 0:1280].rearrange("p (h d) -> p h d", d=64)
                        n3 = qkn[:].rearrange("p (h d) -> p h d", d=64)
                        kk.op(dve, lambda: V.tensor_tensor(out=n3, in0=q3, in1=rstd[:].unsqueeze(2).to_broadcast([128, 20, 64]),
                                                           op=ALU.mult), r=[q_, rstd], w=[qkn])
                        kk.op(dve, lambda: V.tensor_tensor(out=n3[:, 0:16, :], in0=n3[:, 0:16, :],
                                                           in1=gq[:].unsqueeze(1).to_broadcast([128, 16, 64]), op=ALU.mult),
                              r=[qkn, gq], w=[qkn])
                        kk.op(dve, lambda: V.tensor_tensor(out=n3[:, 16:20, :], in0=n3[:, 16:20, :],
                                                           in1=gk[:].unsqueeze(1).to_broadcast([128, 4, 64]), op=ALU.mult),
                              r=[qkn, gk], w=[qkn])
                        tix = b * NTS + j
                        cb_ = cosT[:, tix, :].unsqueeze(1).to_broadcast([128, 20, 8])
                        sb_ = sinT[:, tix, :].unsqueeze(1).to_broadcast([128, 20, 8])
                        t1_ = n3[:, :, 0:8]
                        t2_ = n3[:, :, 8:16]
                        kk.op(dve, lambda: V.tensor_tensor(out=r1[:], in0=t1_, in1=cb_, op=ALU.mult), r=[qkn, cosT], w=[r1])
                        kk.op(pool, lambda: G.tensor_tensor(out=r2[:], in0=t2_, in1=sb_, op=ALU.mult), r=[qkn, sinT], w=[r2])
                        kk.op(dve, lambda: V.tensor_tensor(out=r3[:], in0=t2_, in1=cb_, op=ALU.mult), r=[qkn, cosT], w=[r3])
                        kk.op(pool, lambda: G.tensor_tensor(out=r4[:], in0=t1_, in1=sb_, op=ALU.mult), r=[qkn, sinT], w=[r4])
                        kk.op(act, lambda: A.copy(out=q_r[:], in_=qkn[:]), r=[qkn], w=[q_r])
                        qr3 = q_r[:].rearrange("p (h d) -> p h d", d=64)
                        kk.op(dve, lambda: V.tensor_tensor(out=qr3[:, :, 0:8], in0=r1[:], in1=r2[:], op=ALU.subtract),
                              r=[r1, r2, q_r], w=[q_r])
                        kk.op(dve, lambda: V.tensor_tensor(out=qr3[:, :, 8:16], in0=r3[:], in1=r4[:], op=ALU.add),
                              r=[r3, r4, q_r], w=[q_r])
                        pq0 = ptq[(2 * j) % 4]
                        pq1 = ptq[(2 * j + 1) % 4]
                        pk = ptk[j % 2]
                        for h in range(20):
                            dst = pq0 if h < 8 else (pq1 if h < 16 else pk)
                            hh = h % 8 if h < 16 else h - 16
                            kk.op(pe, lambda: PE.transpose(out=dst[:, hh, :], in_=qr3[:, h, :], identity=identb[:]),
                                  r=[q_r, identb], j=[dst])
                        kk.op(act, lambda: A.copy(out=qT[:, 0:8, j * 128:(j + 1) * 128], in_=pq0[:]), r=[pq0], j=[qT])
                        kk.op(dve, lambda: V.tensor_copy(out=qT[:, 8:16, j * 128:(j + 1) * 128], in_=pq1[:]), r=[pq1], j=[qT])
                        kk.op(act, lambda: A.copy(out=kT[:, :, j * 128:(j + 1) * 128], in_=pk[:]), r=[pk], j=[kT])
                        kk.op(pool, lambda: G.tensor_copy(out=vau[:, j, :, 0:64],
                                                          in_=q_[:, 1280:1536].rearrange("p (g d) -> p g d", d=64)),
                              r=[q_], j=[vau])
                    kk.barrier()
                with contextlib.ExitStack() as st2:
                    sc = [ps(st2, "sc%d" % i, [128, 512]) for i in range(4)]
                    ops_ = [ps(st2, "ops%d" % i, [128, 4, 65]) for i in range(2)]
                    pta = ps(st2, "pta", [128, 8, 128], BF16)
                    pT = [sb(st2, "pT%d" % i, [128, 512], BF16) for i in range(6)]
                    den = sb(st2, "den", [128, 4])
                    ao = [sb(st2, "ao%d" % i, [128, 1024], BF16) for i in range(2)]
                    cnt = 0
                    gcnt = 0
                    for j in range(NTS):
                        a_o = ao[j % 2]
                        ao3 = a_o[:].rearrange("p (h d) -> p h d", d=64)
                        for g in range(4):
                            jjs = [jj for jj in (j - 1, j, j + 1) if 0 <= jj < NTS]
                            pts = []
                            for jj in jjs:
                                s_ = sc[cnt % 4]
                                p_ = pT[cnt % 6]
                                cnt += 1
                                kk.op(pe, lambda: PE.matmul(s_[:], lhsT=kT[:, g, jj * 128:(jj + 1) * 128],
                                                            rhs=qT[:, 4 * g:4 * g + 4, j * 128:(j + 1) * 128],
                                                            start=True, stop=True), r=[kT, qT], w=[s_])
                                kk.op(act, lambda: A.activation(out=p_[:], in_=s_[:], func=AF.Exp), r=[s_], w=[p_])
                                if jj != j:
                                    mk = triB if jj < j else triF
                                    p3 = p_[:].rearrange("p (h q) -> p h q", q=128)
                                    kk.op(dve, lambda: V.tensor_tensor(out=p3, in0=p3,
                                                                       in1=mk[:].unsqueeze(1).to_broadcast([128, 4, 128]),
                                                                       op=ALU.mult), r=[p_, mk], w=[p_])
                                pts.append((jj, p_))
                            o_ = ops_[gcnt % 2]
                            gcnt += 1
                            for h in range(4):
                                for ii, (jj, p_) in enumerate(pts):
                                    kk.op(pe, lambda: PE.matmul(o_[:, h, :], lhsT=p_[:, h * 128:(h + 1) * 128],
                                                                rhs=vau[:, jj, g, :], start=(ii == 0), stop=(ii == len(pts) - 1)),
                                          r=[p_, vau], j=[o_])
                            kk.op(dve, lambda: V.tensor_tensor(out=den[:], in0=o_[:, :, 64], in1=esk[:, 4 * g:4 * g + 4], op=ALU.add),
                                  r=[o_, esk], w=[den])
                            kk.op(dve, lambda: V.reciprocal(out=den[:], in_=den[:]), r=[den], w=[den])
                            kk.op(dve, lambda: V.tensor_tensor(out=ao3[:, 4 * g:4 * g + 4, :], in0=o_[:, :, 0:64],
                                                               in1=den[:].unsqueeze(2).to_broadcast([128, 4, 64]), op=ALU.mult),
                                  r=[o_, den], j=[a_o])
                        for k in range(8):
                            kk.op(pe, lambda: PE.transpose(out=pta[:, k, :], in_=a_o[:, k * 128:(k + 1) * 128], identity=identb[:]),
                                  r=[a_o, identb], j=[pta])
                        kk.op(act, lambda: A.copy(out=aoT[:, :, j * 128:(j + 1) * 128], in_=pta[:]), r=[pta], j=[aoT])
                    kk.barrier()

        def phase_ssd(l, b, ysT):
            with contextlib.ExitStack() as st:
                dtb = sb(st, "dtb", [128, 64])
                aneg = sb(st, "aneg", [128, 64])
                dsk = sb(st, "dsk", [128, 32])
                ngs = sb(st, "ngs", [128, 2048])
                bc_load(sp, dtb, dt_bias[l:l + 1, :])
                bc_load(sp, aneg, a_log[l:l + 1, :])
                bc_load(sp, dsk, ssm_d[l:l + 1, :])
                bc_load(sp, ngs, ssm_norm_g[l:l + 1, :])
                kk.op(act, lambda: A.activation(out=aneg[:], in_=aneg[:], func=AF.Exp), r=[aneg], w=[aneg])
                kk.op(dve, lambda: V.tensor_scalar(out=aneg[:], in0=aneg[:], scalar1=-1.0, scalar2=None, op0=ALU.mult),
                      r=[aneg], w=[aneg])
                xs = sb(st, "xs", [128, 2048])
                dtr = sb(st, "dtr", [128, 64])
                dt_ = sb(st, "dt", [128, 64])
                da = sb(st, "da", [128, 64])
                cpsb = sb(st, "cpsb", [128, 128])
                ecum = sb(st, "ecum", [128, 64])
                edte = sb(st, "edte", [128, 64])
                etot = sb(st, "etot", [128, 64])
                w2 = sb(st, "w2", [128, 64])
                xdt = [sb(st, "xdt%d" % d, [128, 2048], BF16) for d in range(2)]
                xdte = [sb(st, "xdte%d" % d, [128, 2048], BF16) for d in range(2)]
                Hs = [sb(st, "H%d" % d, [128, 2048]) for d in range(2)]
                Hb = [sb(st, "Hb%d" % d, [128, 2048], BF16) for d in range(2)]
                btc = sb(st, "btc", [128, 4, 128], BF16)
                ctc = sb(st, "ctc", [128, 4, 128], BF16)
                btm = sb(st, "btm", [128, 512], BF16)
                cps = ps(st, "cps", [128, 128])
                big = [ps(st, "big%d" % i, [128, 512]) for i in range(4)]
                bigc = [0]

                def nbig():
                    bigc[0] += 1
                    return big[bigc[0] % 4]

                def prep(c, dirs):
                    r0 = b * S + c * 128
                    kk.dma(sp, xs[:], xs_tm[r0:r0 + 128, :], w=[xs])
                    kk.dma(sp, dtr[:], proj_tm[r0:r0 + 128, 3584:3648], w=[dtr])
                    kk.dma(sp, btm[:], btm_d[r0:r0 + 128, :], w=[btm])
                    kk.op(dve, lambda: V.tensor_tensor(out=dt_[:], in0=dtr[:], in1=dtb[:], op=ALU.add), r=[dtr, dtb], w=[dt_])
                    kk.op(act, lambda: A.activation(out=dt_[:], in_=dt_[:], func=AF.Exp), r=[dt_], w=[dt_])
                    kk.op(act, lambda: A.activation(out=dt_[:], in_=dt_[:], func=AF.Ln, bias=oneb[:, 0:1]), r=[dt_, oneb], w=[dt_])
                    kk.op(dve, lambda: V.tensor_tensor(out=da[:], in0=dt_[:], in1=aneg[:], op=ALU.mult), r=[dt_, aneg], w=[da])
                    if XP == 1:
                        return
                    kk.op(pe, lambda: PE.matmul(cps[:, 0:32], lhsT=triF[:], rhs=da[:, 0:32], start=True, stop=True),
                          r=[triF, da], w=[cps])
                    kk.op(pe, lambda: PE.matmul(cps[:, 32:64], lhsT=triB[:], rhs=da[:, 32:64], start=True, stop=True),
                          r=[triB, da], j=[cps])
                    kk.op(pe, lambda: PE.matmul(cps[:, 64:128], lhsT=ones_f[:], rhs=da[:], start=True, stop=True),
                          r=[ones_f, da], j=[cps])
                    if XP == 3:
                        return
                    kk.op(dve, lambda: V.tensor_copy(out=cpsb[:], in_=cps[:]), r=[cps], w=[cpsb])
                    kk.op(act, lambda: A.activation(out=ecum[:], in_=cpsb[:, 0:64], func=AF.Exp), r=[cpsb], w=[ecum])
                    kk.op(act, lambda: A.activation(out=etot[:], in_=cpsb[:, 64:128], func=AF.Exp), r=[cpsb], w=[etot])
                    kk.op(dve, lambda: V.tensor_tensor(out=edte[:], in0=cpsb[:, 64:128], in1=cpsb[:, 0:64], op=ALU.subtract),
                          r=[cpsb], w=[edte])
                    kk.op(act, lambda: A.activation(out=edte[:], in_=edte[:], func=AF.Exp), r=[edte], w=[edte])
                    kk.op(dve, lambda: V.tensor_tensor(out=w2[:], in0=dt_[:], in1=edte[:], op=ALU.mult), r=[dt_, edte], w=[w2])
                    if XP == 4:
                        return
                    x3 = xs[:].rearrange("p (h d) -> p h d", d=64)
                    main = len(dirs) == 2
                    for d in (dirs if main else []):
                        kk.op(pool, lambda: G.tensor_tensor(out=xdt[d][:].rearrange("p (h d) -> p h d", d=64), in0=x3,
                                                            in1=dt_[:, d * 32:(d + 1) * 32].unsqueeze(2).to_broadcast([128, 32, 64]),
                                                            op=ALU.mult), r=[xs, dt_], w=[xdt[d]])
                    for d in ([0] if main else [1]):
                        kk.op(pool, lambda: G.tensor_tensor(out=xdte[d][:].rearrange("p (h d) -> p h d", d=64), in0=x3,
                                                            in1=w2[:, d * 32:(d + 1) * 32].unsqueeze(2).to_broadcast([128, 32, 64]),
                                                            op=ALU.mult), r=[xs, w2], w=[xdte[d]])

                def state_update(c, d):
                    H = Hs[d]
                    kk.op(dve, lambda: V.tensor_tensor(out=H[:].rearrange("p (h d) -> p h d", d=64),
                                                       in0=H[:].rearrange("p (h d) -> p h d", d=64),
                                                       in1=etot[:, d * 32:(d + 1) * 32].unsqueeze(2).to_broadcast([128, 32, 64]),
                                                       op=ALU.mult), r=[H, etot], w=[H])
                    for g in range(4):
                        sp_ = nbig()
                        kk.op(pe, lambda: PE.matmul(sp_[:], lhsT=btm[:, g * 128:(g + 1) * 128],
                                                    rhs=xdte[d][:, g * 512:(g + 1) * 512], start=True, stop=True),
                              r=[btm, xdte[d]], w=[sp_])
                        kk.op(dve, lambda: V.tensor_tensor(out=H[:, g * 512:(g + 1) * 512], in0=H[:, g * 512:(g + 1) * 512],
                                                           in1=sp_[:], op=ALU.add), r=[H, sp_], w=[H])

                kk.op(pool, lambda: G.memset(Hs[1][:], 0.0), w=[Hs[1]])
                for c in range(NTS - 1, -1, -1):
                    kk.op(act, lambda: A.copy(out=Hb[1][:], in_=Hs[1][:]), r=[Hs[1]], w=[Hb[1]])
                    kk.dma(sp, gst_d[b, c, :, :], Hb[1][:], r=[Hb[1]])
                    if c > 0 and XP >= 1:
                        prep(c, [1])
                        if XP == 2:
                            state_update(c, 1)
                kk.barrier()
                if stop == "ssd_pre":
                    return

                with contextlib.ExitStack() as st2:
                    cbp = ps(st2, "cbp", [128, 4, 128])
                    yps = ps(st2, "yps", [128, 512])
                    pty = ps(st2, "pty", [128, 8, 128], BF16)
                    cbm = [sb(st2, "cbm%d" % d, [128, 4, 128]) for d in range(2)]
                    rhsD = [sb(st2, "rhsD%d" % d, [128, 8, 128]) for d in range(2)]
                    ex = [sb(st2, "ex%d" % i, [128, 512]) for i in range(2)]
                    MT = [sb(st2, "MT%d" % d, [128, 8, 128], BF16) for d in range(2)]
                    yy = sb(st2, "yy", [128, 2048])
                    zz = sb(st2, "zz", [128, 2048])
                    tA = sb(st2, "tA", [128, 512])
                    tB = sb(st2, "tB", [128, 512])
                    tC = sb(st2, "tC", [128, 512])
                    ssy = sb(st2, "ssy", [128, 4])
                    ynb = xdt[0]
                    kk.op(pool, lambda: G.memset(Hs[0][:], 0.0), w=[Hs[0]])
                    kk.op(pool, lambda: G.memset(Hb[0][:], 0.0), w=[Hb[0]])
                    Us = [sgtF, sltB]
                    Ts = [triF, triB]
                    exc = 0
                    for c in range(NTS):
                        r0 = b * S + c * 128
                        cs = slice(r0, r0 + 128)
                        prep(c, [0, 1])
                        kk.dma(sp, Hb[1][:], gst_d[b, c, :, :], w=[Hb[1]])
                        kk.dma(sp, zz[:], proj_tm[r0:r0 + 128, 1536:3584], w=[zz])
                        for g in range(4):
                            kk.dma(sp, btc[:, g, :], bt_d[g, :, cs], j=[btc])
                            kk.dma(sp, ctc[:, g, :], ct_d[g, :, cs], j=[ctc])
                        for g in range(4):
                            kk.op(pe, lambda: PE.matmul(cbp[:, g, :], lhsT=btc[:, g, :], rhs=ctc[:, g, :], start=True, stop=True),
                                  r=[btc, ctc], j=[cbp])
                        for d in range(2):
                            kk.op(dve, lambda: V.tensor_tensor(out=cbm[d][:], in0=cbp[:],
                                                               in1=Ts[d][:].unsqueeze(1).to_broadcast([128, 4, 128]), op=ALU.mult),
                                  r=[cbp, Ts[d]], w=[cbm[d]])
                        for g in range(4):
                            for d in range(2):
                                kk.op(pool, lambda: G.tensor_tensor(out=rhsD[d][:],
                                                                    in0=Ts[d][:].unsqueeze(1).to_broadcast([128, 8, 128]),
                                                                    in1=da[:, d * 32 + g * 8:d * 32 + g * 8 + 8].unsqueeze(2).to_broadcast([128, 8, 128]),
                                                                    op=ALU.mult), r=[Ts[d], da], w=[rhsD[d]])
                                for hf in range(2):
                                    dp = nbig()
                                    e_ = ex[exc % 2]
                                    exc += 1
                                    kk.op(pe, lambda: PE.matmul(dp[:], lhsT=Us[d][:], rhs=rhsD[d][:, hf * 4:hf * 4 + 4, :],
                                                                start=True, stop=True), r=[Us[d], rhsD[d]], w=[dp])
                                    kk.op(act, lambda: A.activation(out=e_[:], in_=dp[:], func=AF.Exp), r=[dp], w=[e_])
                                    kk.op(dve, lambda: V.tensor_tensor(out=MT[d][:, hf * 4:hf * 4 + 4, :],
                                                                       in0=e_[:].rearrange("p (h l) -> p h l", l=128),
                                                                       in1=cbm[d][:, g, :].unsqueeze(1).to_broadcast([128, 4, 128]),
                                                                       op=ALU.mult), r=[e_, cbm[d]], j=[MT[d]])
                            for h8 in range(8):
                                h = g * 8 + h8
                                for d in range(2):
                                    kk.op(pe, lambda: PE.matmul(yps[:, h8 * 64:(h8 + 1) * 64], lhsT=MT[d][:, h8, :],
                                                                rhs=xdt[d][:, h * 64:(h + 1) * 64], start=(d == 0), stop=(d == 1)),
                                          r=[MT[d], xdt[d]], j=[yps])
                            yo = [nbig(), nbig()]
                            for d in range(2):
                                kk.op(pe, lambda: PE.matmul(yo[d][:], lhsT=ctc[:, g, :], rhs=Hb[d][:, g * 512:(g + 1) * 512],
                                                            start=True, stop=True), r=[ctc, Hb[d]], w=[yo[d]])
                            gs = slice(g * 512, (g + 1) * 512)

                            def bc8(t, d):
                                return t[:, d * 32 + g * 8:d * 32 + g * 8 + 8].unsqueeze(2).to_broadcast([128, 8, 64])
                            v3 = lambda t: t[:].rearrange("p (h d) -> p h d", d=64)
                            kk.op(dve, lambda: V.tensor_tensor(out=v3(tA), in0=v3(yo[0]), in1=bc8(ecum, 0), op=ALU.mult),
                                  r=[yo[0], ecum], w=[tA])
                            kk.op(dve, lambda: V.tensor_tensor(out=v3(tB), in0=v3(yo[1]), in1=bc8(ecum, 1), op=ALU.mult),
                                  r=[yo[1], ecum], w=[tB])
                            kk.op(pool, lambda: G.tensor_tensor(out=v3(tC), in0=xs[:, gs].rearrange("p (h d) -> p h d", d=64),
                                                                in1=dsk[:, g * 8:g * 8 + 8].unsqueeze(2).to_broadcast([128, 8, 64]),
                                                                op=ALU.mult), r=[xs, dsk], w=[tC])
                            kk.op(dve, lambda: V.tensor_tensor(out=tA[:], in0=tA[:], in1=yps[:], op=ALU.add), r=[tA, yps], w=[tA])
                            kk.op(pool, lambda: G.tensor_tensor(out=tB[:], in0=tB[:], in1=tC[:], op=ALU.add), r=[tB, tC], w=[tB])
                            kk.op(dve, lambda: V.tensor_tensor(out=yy[:, gs], in0=tA[:], in1=tB[:], op=ALU.add), r=[tA, tB], j=[yy])
                        if stop == "ssd_nofin":
                            continue
                        state_update(c, 0)
                        kk.op(act, lambda: A.copy(out=Hb[0][:], in_=Hs[0][:]), r=[Hs[0]], w=[Hb[0]])
                        kk.op(act, lambda: A.activation(out=zz[:], in_=zz[:], func=AF.Silu), r=[zz], w=[zz])
                        kk.op(dve, lambda: V.tensor_tensor(out=yy[:], in0=yy[:], in1=zz[:], op=ALU.mult), r=[yy, zz], w=[yy])
                        for g in range(4):
                            kk.op(act, lambda: A.activation(out=zz[:, g * 512:(g + 1) * 512], in_=yy[:, g * 512:(g + 1) * 512],
                                                            func=AF.Square, accum_out=ssy[:, g:g + 1]), r=[yy], w=[zz, ssy] if g == 0 else (), j=() if g == 0 else [zz, ssy])
                        kk.op(act, lambda: A.activation(out=ssy[:], in_=ssy[:], func=AF.Sqrt, scale=1.0 / 512, bias=epsb[:, 0:1]),
                              r=[ssy, epsb], w=[ssy])
                        kk.op(dve, lambda: V.reciprocal(out=ssy[:], in_=ssy[:]), r=[ssy], w=[ssy])
                        kk.op(dve, lambda: V.tensor_tensor(out=yy[:].rearrange("p (g d) -> p g d", d=512),
                                                           in0=yy[:].rearrange("p (g d) -> p g d", d=512),
                                                           in1=ssy[:].unsqueeze(2).to_broadcast([128, 4, 512]), op=ALU.mult),
                              r=[yy, ssy], w=[yy])
                        kk.op(pool, lambda: G.tensor_tensor(out=ynb[:], in0=yy[:], in1=ngs[:], op=ALU.mult), r=[yy, ngs], w=[ynb])
                        for half in range(2):
                            for k in range(8):
                                kc = half * 8 + k
                                kk.op(pe, lambda: PE.transpose(out=pty[:, k, :], in_=ynb[:, kc * 128:(kc + 1) * 128], identity=identb[:]),
                                      r=[ynb, identb], j=[pty])
                            kk.op(act, lambda: A.copy(out=ysT[:, half * 8:half * 8 + 8, c * 128:(c + 1) * 128], in_=pty[:]),
                                  r=[pty], j=[ysT])
                    kk.barrier()

        def phase_outproj(l, b, aoT, ysT):
            with contextlib.ExitStack() as st:
                wao = sb(st, "wao", [128, 8, D], BF16)
                wso = sb(st, "wso", [128, 16, D], BF16)
                wo = sb(st, "wo", [128, 8, D], BF16)
                g1 = sb(st, "g1bc", [128, D])
                kk.dma(pool, wao[:], w_attn_o[l, :, :].rearrange("(k p) n -> p k n", p=128), w=[wao])
                kk.dma(pool, wso[:], w_ssm_o[l, :, :].rearrange("(k p) n -> p k n", p=128), w=[wso])
                kk.dma(pool, wo[:], w_out[l, :, :].rearrange("(k p) n -> p k n", p=128), w=[wo])
                bc_load(sp, g1, modd[l, b:b + 1, 2 * D:3 * D])
                mT = sb(st, "mT", [128, 8, TG], BF16)
                gA = [sb(st, "gA%d" % i, [128, TG]) for i in range(2)]
                gS = [sb(st, "gS%d" % i, [128, TG]) for i in range(2)]
                t1 = [sb(st, "mt1%d" % i, [128, TG]) for i in range(2)]
                t2 = [sb(st, "mt2%d" % i, [128, TG]) for i in range(2)]
                xt = [sb(st, "oxt%d" % i, [128, D]) for i in range(1)]
                xn = [sb(st, "oxn%d" % i, [128, D]) for i in range(1)]
                tmp = sb(st, "otmp", [128, 512])
                pa = [ps(st, "pa%d" % i, [128, TG]) for i in range(2)]
                pss = [ps(st, "pss%d" % i, [128, TG]) for i in range(2)]
                po = [ps(st, "po%d" % i, [128, 512]) for i in range(2)]
                mc = 0
                tc_ = 0
                for tg in range(NTG):
                    tl = tg * TG
                    t0 = b * S + tl
                    for m in range(8):
                        p_a = pa[mc % 2]
                        p_s = pss[mc % 2]
                        g_a = gA[mc % 2]
                        g_s = gS[mc % 2]
                        t_1 = t1[mc % 2]
                        t_2 = t2[mc % 2]
                        mc += 1
                        kk.dma(sp, g_a[:], gT_d[m * 128:(m + 1) * 128, t0:t0 + TG], w=[g_a])
                        kk.dma(sp, g_s[:], gT_d[1024 + m * 128:1024 + (m + 1) * 128, t0:t0 + TG], w=[g_s])
                        for k in range(8):
                            kk.op(pe, lambda: PE.matmul(p_a[:], lhsT=wao[:, k, m * 128:(m + 1) * 128], rhs=aoT[:, k, tl:tl + TG],
                                                        start=(k == 0), stop=(k == 7)), r=[wao, aoT], j=[p_a])
                        for k in range(16):
                            kk.op(pe, lambda: PE.matmul(p_s[:], lhsT=wso[:, k, m * 128:(m + 1) * 128], rhs=ysT[:, k, tl:tl + TG],
                                                        start=(k == 0), stop=(k == 15)), r=[wso, ysT], j=[p_s])
                        kk.op(dve, lambda: V.tensor_tensor(out=t_1[:], in0=p_a[:], in1=g_a[:], op=ALU.mult), r=[p_a, g_a], w=[t_1])
                        kk.op(dve, lambda: V.tensor_tensor(out=t_2[:], in0=p_s[:], in1=g_s[:], op=ALU.mult), r=[p_s, g_s], w=[t_2])
                        kk.op(pool, lambda: G.tensor_tensor(out=mT[:, m, :], in0=t_1[:], in1=t_2[:], op=ALU.add),
                              r=[t_1, t_2], j=[mT])
                    for i in range(TG // 128):
                        r0 = t0 + i * 128
                        x_t = xt[0]
                        x_n = xn[0]
                        tc_ += 1
                        kk.dma(sp, x_t[:], xres[r0:r0 + 128, :], w=[x_t])
                        for n in range(2):
                            p_o = po[n]
                            for m in range(8):
                                kk.op(pe, lambda: PE.matmul(p_o[:], lhsT=mT[:, m, i * 128:(i + 1) * 128], rhs=wo[:, m, n * 512:(n + 1) * 512],
                                                            start=(m == 0), stop=(m == 7)), r=[mT, wo], j=[p_o])
                            kk.op(dve, lambda: V.tensor_tensor(out=tmp[:], in0=p_o[:], in1=g1[:, n * 512:(n + 1) * 512], op=ALU.mult),
                                  r=[p_o, g1], w=[tmp])
                            kk.op(pool, lambda: G.tensor_tensor(out=x_n[:, n * 512:(n + 1) * 512], in0=tmp[:],
                                                                in1=x_t[:, n * 512:(n + 1) * 512], op=ALU.add),
                                  r=[tmp, x_t], j=[x_n])
                        kk.dma(sp, xres[r0:r0 + 128, :], x_n[:], r=[x_n])
                kk.barrier()

        def phase_moe(l, final):
            dst = y_out if final else xres
            for b in range(NB):
                with contextlib.ExitStack() as st:
                    h2T = sb(st, "h2T", [128, 8, S], BF16)
                    comb = sb(st, "comb", [128, NTS, NE])
                    accs = [sb(st, "acc%d" % j, [128, D]) for j in range(NTS)]
                    bg = sb(st, "bg", [128, NE, 8])
                    bu = sb(st, "bu", [128, NE, 8])
                    kk.dma(sp, bg[:], b_gate[l, :, :, :], w=[bg])
                    kk.dma(sp, bu[:], b_up[l, :, :, :], w=[bu])
                    kk.op(dve, lambda: V.tensor_scalar(out=bu[:], in0=bu[:], scalar1=1.0, scalar2=None, op0=ALU.add), r=[bu], w=[bu])
                    with contextlib.ExitStack() as st1:
                        ng = sb(st1, "ng2", [128, D])
                        G2 = sb(st1, "G2", [128, D])
                        SH2 = sb(st1, "SH2", [128, D])
                        bc_load(sp, ng, norm2_g[l:l + 1, :])
                        bc_load(sp, G2, modd[l, b:b + 1, 4 * D:5 * D])
                        bc_load(sp, SH2, modd[l, b:b + 1, 3 * D:4 * D])
                        kk.op(dve, lambda: V.tensor_tensor(out=G2[:], in0=G2[:], in1=ng[:], op=ALU.mult), r=[G2, ng], w=[G2])
                        rw = sb(st1, "rw", [128, 8, NE])
                        rb = sb(st1, "rb", [128, NE])
                        bd = sb(st1, "bd", [NE, D])
                        kk.dma(sp, rw[:], router_w[l, :, :].rearrange("(k p) e -> p k e", p=128), w=[rw])
                        bc_load(sp, rb, router_b[l:l + 1, :])
                        kk.dma(sp, bd[:], b_down[l, :, :], w=[bd])
                        xt = [sb(st1, "mxt%d" % i, [128, D]) for i in range(2)]
                        hf = [sb(st1, "mhf%d" % i, [128, D]) for i in range(2)]
                        hb = [sb(st1, "mhb%d" % i, [128, D], BF16) for i in range(2)]
                        hTf2 = [sb(st1, "hTf%d" % i, [128, 8, 128]) for i in range(2)]
                        lg2 = [sb(st1, "lg%d" % i, [128, NE]) for i in range(2)]
                        mx82 = [sb(st1, "mx8%d" % i, [128, 8]) for i in range(2)]
                        msk2 = [sb(st1, "msk%d" % i, [128, NE]) for i in range(2)]
                        nmx2 = [sb(st1, "nmx%d" % i, [128, 1]) for i in range(2)]
                        ee2 = [sb(st1, "ee%d" % i, [128, NE]) for i in range(2)]
                        dn2 = [sb(st1, "dn%d" % i, [128, 1]) for i in range(2)]
                        cT2 = [sb(st1, "cT%d" % i, [NE, 128]) for i in range(2)]
                        sq2 = [sb(st1, "msq%d" % i, [128, D]) for i in range(2)]
                        ss2 = [sb(st1, "mss%d" % i, [128, 4]) for i in range(2)]
                        ptr = [ps(st1, "mptr%d" % i, [128, 8, 128], BF16) for i in range(2)]
                        ptf = [ps(st1, "mptf%d" % i, [128, 4, 128]) for i in range(2)]
                        plg = ps(st1, "plg", [128, NE])
                        pct = ps(st1, "pct", [NE, 128])
                        pbd = [ps(st1, "pbd%d" % i, [128, 512]) for i in range(2)]
                        for j in range(NTS):
                            r0 = b * S + j * 128
                            x_t = xt[j % 2]
                            h_f = hf[j % 2]
                            h_b = hb[j % 2]
                            p_t = ptr[j % 2]
                            hTf, lg, mx8, msk, nmx, ee, dn, cT, sq, ss = (hTf2[j % 2], lg2[j % 2], mx82[j % 2], msk2[j % 2], nmx2[j % 2],
                                                                         ee2[j % 2], dn2[j % 2], cT2[j % 2], sq2[j % 2], ss2[j % 2])
                            kk.dma(sp, x_t[:], xres[r0:r0 + 128, :], w=[x_t])
                            kk.op(act, lambda: A.activation(out=sq[:], in_=x_t[:], func=AF.Square, accum_out=ss[:, 0:1]),
                                  r=[x_t], w=[sq, ss])
                            kk.op(act, lambda: A.activation(out=ss[:, 1:2], in_=ss[:, 0:1], func=AF.Sqrt, scale=1.0 / D, bias=epsb[:, 0:1]),
                                  r=[ss, epsb], w=[ss])
                            kk.op(dve, lambda: V.reciprocal(out=ss[:, 2:3], in_=ss[:, 1:2]), r=[ss], w=[ss])
                            kk.op(dve, lambda: V.scalar_tensor_tensor(out=sq[:], in0=x_t[:], scalar=ss[:, 2:3], in1=G2[:],
                                                                      op0=ALU.mult, op1=ALU.mult), r=[x_t, ss, G2], w=[sq])
                            kk.op(pool, lambda: G.tensor_tensor(out=h_f[:], in0=sq[:], in1=SH2[:], op=ALU.add), r=[sq, SH2], w=[h_f])
                            kk.op(act, lambda: A.copy(out=h_b[:], in_=h_f[:]), r=[h_f], w=[h_b])
                            for k in range(8):
                                kk.op(pe, lambda: PE.transpose(out=p_t[:, k, :], in_=h_b[:, k * 128:(k + 1) * 128], identity=identb[:]),
                                      r=[h_b, identb], j=[p_t])
                            kk.op(act, lambda: A.copy(out=h2T[:, :, j * 128:(j + 1) * 128], in_=p_t[:]), r=[p_t], j=[h2T])
                            for half in range(2):
                                pf = ptf[half]
                                for k in range(4):
                                    kc = half * 4 + k
                                    kk.op(pe, lambda: PE.transpose(out=pf[:, k, :], in_=h_f[:, kc * 128:(kc + 1) * 128], identity=identf[:]),
                                          r=[h_f, identf], j=[pf])
                                kk.op(dve, lambda: V.tensor_copy(out=hTf[:, half * 4:half * 4 + 4, :], in_=pf[:]), r=[pf], j=[hTf])
                            for k in range(8):
                                kk.op(pe, lambda: PE.matmul(plg[:], lhsT=hTf[:, k, :], rhs=rw[:, k, :], start=(k == 0), stop=(k == 7)),
                                      r=[hTf, rw], j=[plg])
                            kk.op(dve, lambda: V.tensor_tensor(out=lg[:], in0=plg[:], in1=rb[:], op=ALU.add), r=[plg, rb], w=[lg])
                            kk.op(dve, lambda: V.max(out=mx8[:], in_=lg[:]), r=[lg], w=[mx8])
                            kk.op(dve, lambda: V.tensor_scalar(out=msk[:], in0=lg[:], scalar1=mx8[:, 3:4], scalar2=None, op0=ALU.is_ge),
                                  r=[lg, mx8], w=[msk])
                            kk.op(dve, lambda: V.tensor_scalar(out=nmx[:], in0=mx8[:, 0:1], scalar1=-1.0, scalar2=None, op0=ALU.mult),
                                  r=[mx8], w=[nmx])
                            kk.op(act, lambda: A.activation(out=ee[:], in_=lg[:], func=AF.Exp, bias=nmx[:, 0:1]), r=[lg, nmx], w=[ee])
                            kk.op(dve, lambda: V.tensor_tensor(out=ee[:], in0=ee[:], in1=msk[:], op=ALU.mult), r=[ee, msk], w=[ee])
                            kk.op(dve, lambda: V.tensor_reduce(out=dn[:], in_=ee[:], axis=AX.X, op=ALU.add), r=[ee], w=[dn])
                            kk.op(dve, lambda: V.reciprocal(out=dn[:], in_=dn[:]), r=[dn], w=[dn])
                            kk.op(dve, lambda: V.tensor_scalar(out=comb[:, j, :], in0=ee[:], scalar1=dn[:, 0:1], scalar2=None, op0=ALU.mult),
                                  r=[ee, dn], j=[comb])
                            kk.op(dve, lambda: V.tensor_scalar(out=msk[:], in0=ee[:], scalar1=dn[:, 0:1], scalar2=None, op0=ALU.mult),
                                  r=[ee, dn], w=[msk])
                            kk.op(pe, lambda: PE.transpose(out=pct[:], in_=msk[:], identity=identf[:]), r=[msk, identf], w=[pct])
                            kk.op(dve, lambda: V.tensor_copy(out=cT[:], in_=pct[:]), r=[pct], w=[cT])
                            for n in range(2):
                                kk.op(pe, lambda: PE.matmul(pbd[n][:], lhsT=cT[:], rhs=bd[:, n * 512:(n + 1) * 512], start=True, stop=True),
                                      r=[cT, bd], w=[pbd[n]])
                                kk.op(dve, lambda: V.tensor_copy(out=accs[j][:, n * 512:(n + 1) * 512], in_=pbd[n][:]), r=[pbd[n]], j=[accs[j]])
                        kk.barrier()
                    if debug and b == 0 and l == 0:
                        kk.dma(sp, dbg_comb[:, :, :], comb[:], r=[comb])
                    with contextlib.ExitStack() as st2:
                        NWB = 4
                        wbuf = [sb(st2, "wexp%d" % i, [128, 8, D], BF16) for i in range(NWB)]
                        actT = [sb(st2, "actT%d" % i, [128, 8, TG], BF16) for i in range(2)]
                        gt = [sb(st2, "gt%d" % i, [128, TG]) for i in range(2)]
                        sg = [sb(st2, "sg%d" % i, [128, TG]) for i in range(2)]
                        lt = [sb(st2, "lt%d" % i, [128, TG]) for i in range(2)]
                        pg = [ps(st2, "pg%d" % i, [128, TG]) for i in range(2)]
                        pu = [ps(st2, "pu%d" % i, [128, TG]) for i in range(2)]
                        pd = [ps(st2, "pd%d" % i, [128, 512]) for i in range(3)]
                        wsrc = [w_gate, w_up, w_down]
                        wi = [0]
                        loaded = {}

                        def load_w(e, which):
                            wb_ = wbuf[wi[0] % NWB]
                            wi[0] += 1
                            kk.dma(pool, wb_[:], wsrc[which][l, e, :, :].rearrange("(k p) n -> p k n", p=128), w=[wb_])
                            loaded[(e, which)] = wb_

                        for which in range(3):
                            load_w(0, which)
                        mc = 0
                        dc = 0
                        for e in range(NE):
                            wg_, wu_, wd_ = loaded[(e, 0)], loaded[(e, 1)], loaded[(e, 2)]
                            for tg in range(NTG):
                                tl = tg * TG
                                a_T = actT[(e * NTG + tg) % 2]
                                for m in range(8):
                                    p_g = pg[mc % 2]
                                    p_u = pu[mc % 2]
                                    g_t = gt[mc % 2]
                                    s_g = sg[mc % 2]
                                    l_t = lt[mc % 2]
                                    mc += 1
                                    for k in range(8):
                                        kk.op(pe, lambda: PE.matmul(p_g[:], lhsT=wg_[:, k, m * 128:(m + 1) * 128], rhs=h2T[:, k, tl:tl + TG],
                                                                    start=(k == 0), stop=(k == 7)), r=[wg_, h2T], j=[p_g])
                                    for k in range(8):
                                        kk.op(pe, lambda: PE.matmul(p_u[:], lhsT=wu_[:, k, m * 128:(m + 1) * 128], rhs=h2T[:, k, tl:tl + TG],
                                                                    start=(k == 0), stop=(k == 7)), r=[wu_, h2T], j=[p_u])
                                    kk.op(dve, lambda: V.tensor_scalar(out=g_t[:], in0=p_g[:], scalar1=bg[:, e, m:m + 1], scalar2=7.0,
                                                                       op0=ALU.add, op1=ALU.min), r=[p_g, bg], w=[g_t])
                                    kk.op(act, lambda: A.activation(out=s_g[:], in_=g_t[:], func=AF.Sigmoid, scale=1.702), r=[g_t], w=[s_g])
                                    kk.op(dve, lambda: V.tensor_scalar(out=l_t[:], in0=p_u[:], scalar1=bu[:, e, m:m + 1], scalar2=8.0,
                                                                       op0=ALU.add, op1=ALU.min), r=[p_u, bu], w=[l_t])
                                    kk.op(dve, lambda: V.tensor_tensor(out=g_t[:], in0=g_t[:], in1=s_g[:], op=ALU.mult), r=[g_t, s_g], w=[g_t])
                                    kk.op(dve, lambda: V.scalar_tensor_tensor(out=a_T[:, m, :], in0=l_t[:], scalar=-6.0, in1=g_t[:],
                                                                              op0=ALU.max, op1=ALU.mult),
                                          r=[g_t, l_t], j=[a_T])
                                if tg == 0 and e + 1 < NE:
                                    load_w(e + 1, 0)
                                if tg == NTG - 1 and e + 1 < NE:
                                    load_w(e + 1, 1)
                                    load_w(e + 1, 2)
                                for i in range(TG // 128):
                                    jt = tg * (TG // 128) + i
                                    for n in range(2):
                                        p_d = pd[dc % 3]
                                        dc += 1
                                        for m in range(8):
                                            kk.op(pe, lambda: PE.matmul(p_d[:], lhsT=a_T[:, m, i * 128:(i + 1) * 128],
                                                                        rhs=wd_[:, m, n * 512:(n + 1) * 512], start=(m == 0), stop=(m == 7)),
                                                  r=[a_T, wd_], j=[p_d])
                                        kk.op(dve, lambda: V.scalar_tensor_tensor(out=accs[jt][:, n * 512:(n + 1) * 512], in0=p_d[:],
                                                                                  scalar=comb[:, jt, e:e + 1],
                                                                                  in1=accs[jt][:, n * 512:(n + 1) * 512],
                                                                                  op0=ALU.mult, op1=ALU.add),
                                              r=[p_d, comb, accs[jt]], w=[accs[jt]])
                        kk.barrier()
                    with contextlib.ExitStack() as st3:
                        g2 = sb(st3, "g2bc", [128, D])
                        bc_load(sp, g2, modd[l, b:b + 1, 5 * D:6 * D])
                        xt = [sb(st3, "fxt%d" % i, [128, D]) for i in range(2)]
                        xn = [sb(st3, "fxn%d" % i, [128, D]) for i in range(2)]
                        for j in range(NTS):
                            r0 = b * S + j * 128
                            x_t = xt[j % 2]
                            x_n = xn[j % 2]
                            kk.dma(sp, x_t[:], xres[r0:r0 + 128, :], w=[x_t])
                            kk.op(dve, lambda: V.tensor_tensor(out=x_n[:], in0=accs[j][:], in1=g2[:], op=ALU.mult), r=[accs[j], g2], w=[x_n])
                            kk.op(pool, lambda: G.tensor_tensor(out=x_n[:], in0=x_n[:], in1=x_t[:], op=ALU.add), r=[x_n, x_t], w=[x_n])
                            kk.dma(sp, dst[r0:r0 + 128, :], x_n[:], r=[x_n])
                        kk.barrier()

        def phase_mixer(l):
            for b in range(NB):
                with contextlib.ExitStack() as st:
                    aoT = sb(st, "aoT", [128, 8, S], BF16)
                    phase_attn(l, b, aoT)
                    ysT = sb(st, "ysT", [128, 16, S], BF16)
                    if debug and b == 0 and l == 0:
                        kk.dma(sp, dbg_aoT[:, :, :], aoT[:], r=[aoT])
                    if stop == "attn":
                        kk.barrier()
                        continue
                    phase_ssd(l, b, ysT)
                    if debug and b == 0 and l == 0 and stop not in ("ssd_pre", "ssd_nofin"):
                        kk.dma(sp, dbg_ysT[:, :, :], ysT[:], r=[ysT])
                    if stop in ("ssd", "ssd_pre", "ssd_nofin"):
                        kk.barrier()
                        continue
                    phase_outproj(l, b, aoT, ysT)

        oneb = sb(es, "oneb", [128, 1])
        kk.op(pool, lambda: G.memset(oneb[:], 1.0), w=[oneb])

        for l in range(nlayers):
            phase_mod(l)
            if stop == "mod":
                break
            phase_inproj(l)
            if stop == "inproj":
                break
            phase_mixer(l)
            if stop in ("mixer", "attn", "ssd", "ssd_pre", "ssd_nofin"):
                break
            phase_moe(l, final=(l == nlayers - 1))
            if stop == "moe":
                break
        kk.barrier()
        print("instructions:", kk.ninstr)
    return nc


def host_inputs(inputs, NB, S, ncores, ne_decl=NE):
    f = lambda a: np.ascontiguousarray(np.asarray(a))
    x = f(inputs["x"]).astype(np.float32, copy=False)
    c = f(inputs["c"])
    pos = f(inputs["positions"]).astype(np.int32, copy=False)
    Lh = inputs["ada_w"].shape[0]
    shared = {
        "ada_w": f(inputs["ada_w"]), "ada_b": f(inputs["ada_b"]),
        "norm1_g": f(inputs["norm1_g"]), "norm2_g": f(inputs["norm2_g"]),
        "w_in": f(inputs["w_in"]), "q_norm_g": f(inputs["q_norm_g"]), "k_norm_g": f(inputs["k_norm_g"]),
        "attn_sink": f(inputs["attn_sink"]),
        "conv_w": f(np.asarray(inputs["conv_w"]).reshape(Lh, 5, 24, 128).transpose(0, 3, 2, 1)),
        "conv_b": f(np.asarray(inputs["conv_b"]).reshape(Lh, 24, 128).transpose(0, 2, 1)),
        "a_log": f(np.asarray(inputs["a_log"]).reshape(Lh, 64)),
        "dt_bias": f(np.asarray(inputs["dt_bias"]).reshape(Lh, 64)),
        "ssm_d": f(inputs["ssm_d"]), "ssm_norm_g": f(inputs["ssm_norm_g"]),
        "w_attn_o": f(inputs["w_attn_o"]), "w_ssm_o": f(inputs["w_ssm_o"]), "w_out": f(inputs["w_out"]),
        "router_w": f(inputs["router_w"]), "router_b": f(inputs["router_b"]),
        "exp_w_gate": f(np.asarray(inputs["exp_w_gate"])[:, :ne_decl]), "exp_w_up": f(np.asarray(inputs["exp_w_up"])[:, :ne_decl]), "exp_w_down": f(np.asarray(inputs["exp_w_down"])[:, :ne_decl]),
        "exp_b_gate": f(np.asarray(inputs["exp_b_gate"]).reshape(Lh, NE, 8, 128).transpose(0, 3, 1, 2)),
        "exp_b_up": f(np.asarray(inputs["exp_b_up"]).reshape(Lh, NE, 8, 128).transpose(0, 3, 1, 2)),
        "exp_b_down": f(inputs["exp_b_down"]),
    }
    invf = (np.float32(500000.0) ** (-np.arange(0, 16, 2, dtype=np.float32) / np.float32(16))).astype(np.float32)
    shared["invf"] = f(np.broadcast_to(invf[None, :], (128, 8)))
    maps = []
    for i in range(ncores):
        bs = slice(i * NB, (i + 1) * NB)
        m = dict(shared)
        m["x"] = f(x[bs].reshape(NB * S, D))
        m["cT"] = f(c[bs].reshape(NB, 8, 128).transpose(2, 1, 0))
        m["pos"] = f(pos[bs].reshape(NB * S // 128, 128).T)
        maps.append(m)
    return maps


_NC_CACHE = {}


def kernel(**inputs):
    B, S, _ = inputs["x"].shape
    ncores = 8
    NB = B // ncores
    key = (NB, S)
    if key not in _NC_CACHE:
        _NC_CACHE[key] = build(NB, S)
    nc = _NC_CACHE[key]
    maps = host_inputs(inputs, NB, S, ncores)
    res = run_bass_kernel_spmd(nc, maps, core_ids=list(range(ncores)))
    out = np.concatenate([np.asarray(r["y"]).reshape(NB, S, D) for r in res.results], axis=0)
    return out.astype(np.float32, copy=False)
```

```python
import contextlib
import numpy as np
import concourse.bass as bass
import concourse.mybir as mybir
from concourse.bass_utils import run_bass_kernel_spmd

F32 = mybir.dt.float32
BF16 = mybir.dt.bfloat16
I32 = mybir.dt.int32
AF = mybir.ActivationFunctionType
ALU = mybir.AluOpType
AX = mybir.AxisListType

D = 1024
L = 2
NE = 32
EPS = 1e-5
IN_TOTAL = 8768
OQ, OK_, OV, OZ, OXBC, ODT, OG = 0, 1024, 1280, 1536, 3584, 6656, 6720
NTM = 3648
SAME_ENG_SYNC = True
XP = 2


class Buf:
    def __init__(self, t, name=""):
        self.t = t
        self.name = name
        self.writers = []
        self.readers = []
        self.gen_open = False
        self.gen_deps = []

    def __getitem__(self, k):
        return self.t[k]


class Q:
    def __init__(self, kk, eng, name, ndma=0):
        self.eng = eng
        self.name = name
        self.sem = kk.newsem(name)
        self.cnt = 0
        self.waited = {}
        self.dma = [[kk.newsem("%s_d%d" % (name, i)), 0] for i in range(ndma)]
        self.dma_i = 0


class K:
    def __init__(self, nc, es):
        self.nc = nc
        self.es = es
        self.sems = []
        self.pe = Q(self, nc.tensor, "pe")
        self.act = Q(self, nc.scalar, "act", 8)
        self.dve = Q(self, nc.vector, "dve")
        self.pool = Q(self, nc.gpsimd, "pool", 24)
        self.sp = Q(self, nc.sync, "sp", 24)
        self.qs = [self.pe, self.act, self.dve, self.pool, self.sp]
        self.ninstr = 0

    def newsem(self, name):
        s = self.es.enter_context(self.nc.semaphore(name))
        self.sems.append(s)
        return len(self.sems) - 1

    def wait(self, q, tok):
        si, val = tok
        if val <= 0:
            return
        if q.waited.get(si, 0) >= val:
            return
        q.eng.wait_ge(self.sems[si], val)
        q.waited[si] = val
        self.ninstr += 1

    def _deps(self, q, r, w, j):
        deps = []
        for b in r:
            deps += b.writers
        for b in w:
            d = b.writers + b.readers
            deps += d
        for b in j:
            if b.gen_open:
                deps += b.gen_deps
            else:
                d = b.writers + b.readers
                b.gen_deps = d
                deps += d
        for t in set(deps):
            if (not SAME_ENG_SYNC) and t[0] == q.sem:
                continue
            self.wait(q, t)

    def _post(self, tok, r, w, j):
        for b in r:
            b.readers.append(tok)
            b.gen_open = False
        for b in w:
            b.writers = [tok]
            b.readers = []
            b.gen_open = False
        for b in j:
            if b.gen_open:
                b.writers.append(tok)
            else:
                b.writers = [tok]
                b.readers = []
                b.gen_open = True

    def op(self, q, fn, r=(), w=(), j=()):
        self._deps(q, r, w, j)
        ins = fn()
        q.cnt += 1
        ins.then_inc(self.sems[q.sem], 1)
        self.ninstr += 1
        self._post((q.sem, q.cnt), r, w, j)

    def dma(self, q, out, in_, r=(), w=(), j=(), **kw):
        self._deps(q, r, w, j)
        slot = q.dma[q.dma_i % len(q.dma)]
        q.dma_i += 1
        self.wait(q, (slot[0], slot[1]))
        ins = q.eng.dma_start(out=out, in_=in_, **kw)
        slot[1] += 16
        ins.then_inc(self.sems[slot[0]], 16)
        self.ninstr += 1
        self._post((slot[0], slot[1]), r, w, j)

    def barrier(self):
        toks = [(q.sem, q.cnt) for q in self.qs]
        for q in self.qs:
            for s in q.dma:
                toks.append((s[0], s[1]))
        for q in self.qs:
            for t in toks:
                if t[0] == q.sem:
                    continue
                self.wait(q, t)


def build(NB, S, nlayers=L, debug=False, stop=None, ne_decl=NE):
    T = NB * S
    NT = T // 128
    NTS = S // 128
    TG = 512 if S % 512 == 0 else S
    NTG = S // TG
    nc = bass.Bass("TRN2", target_bir_lowering=False)
    dbgkind = "ExternalOutput" if debug else "Internal"

    def din(name, shape, dt=F32):
        return nc.dram_tensor(name, list(shape), dt, kind="ExternalInput").ap()

    def dscr(name, shape, dt=F32):
        return nc.dram_tensor(name, list(shape), dt, kind=dbgkind).ap()

    x_in = din("x", [T, D])
    cT_in = din("cT", [128, 8, NB])
    pos_in = din("pos", [128, NT], I32)
    invf_in = din("invf", [128, 8])
    ada_w = din("ada_w", [L, D, 6 * D])
    ada_b = din("ada_b", [L, 6 * D])
    norm1_g = din("norm1_g", [L, D])
    norm2_g = din("norm2_g", [L, D])
    w_in = din("w_in", [L, D, IN_TOTAL])
    q_norm_g = din("q_norm_g", [L, 64])
    k_norm_g = din("k_norm_g", [L, 64])
    attn_sink = din("attn_sink", [L, 16])
    conv_w = din("conv_w", [L, 128, 24, 5])
    conv_b = din("conv_b", [L, 128, 24])
    a_log = din("a_log", [L, 64])
    dt_bias = din("dt_bias", [L, 64])
    ssm_d = din("ssm_d", [L, 32])
    ssm_norm_g = din("ssm_norm_g", [L, 2048])
    w_attn_o = din("w_attn_o", [L, D, D])
    w_ssm_o = din("w_ssm_o", [L, 2 * D, D])
    w_out = din("w_out", [L, D, D])
    router_w = din("router_w", [L, D, NE])
    router_b = din("router_b", [L, NE])
    w_gate = din("exp_w_gate", [L, ne_decl, D, D])
    b_gate = din("exp_b_gate", [L, 128, NE, 8])
    w_up = din("exp_w_up", [L, ne_decl, D, D])
    b_up = din("exp_b_up", [L, 128, NE, 8])
    w_down = din("exp_w_down", [L, ne_decl, D, D])
    b_down = din("exp_b_down", [L, NE, D])
    y_out = nc.dram_tensor("y", [T, D], F32, kind="ExternalOutput").ap()

    xres = dscr("xres", [T, D])
    modd = dscr("modd", [L, NB, 6 * D])
    proj_tm = dscr("proj_tm", [T, NTM])
    xs_tm = dscr("xs_tm", [T, 2048])
    bt_d = dscr("bt_d", [4, 128, T], BF16)
    ct_d = dscr("ct_d", [4, 128, T], BF16)
    btm_d = dscr("btm_d", [T, 512], BF16)
    gT_d = dscr("gT_d", [2048, T])
    gst_d = dscr("gst_d", [NB, NTS, 128, 2048], BF16)

    dbg_aoT = dscr("dbg_aoT", [128, 8, S], BF16)
    dbg_ysT = dscr("dbg_ysT", [128, 16, S], BF16)
    dbg_comb = dscr("dbg_comb", [128, S // 128, NE])
    es = contextlib.ExitStack()
    with es:
        kk = K(nc, es)
        pe, act, dve, pool, sp = kk.pe, kk.act, kk.dve, kk.pool, kk.sp
        V = nc.vector
        A = nc.scalar
        G = nc.gpsimd
        PE = nc.tensor

        uid = [0]

        def sb(st, name, shape, dt=F32):
            uid[0] += 1
            name = "s%d_%s" % (uid[0], name)
            return Buf(st.enter_context(nc.sbuf_tensor(name, list(shape), dt)), name)

        def ps(st, name, shape, dt=F32):
            uid[0] += 1
            name = "p%d_%s" % (uid[0], name)
            esz = 2 if dt == BF16 else 4
            nel = 1
            for d_ in shape[1:]:
                nel *= d_
            assert nel * esz <= 2048
            t = st.enter_context(nc.psum_tensor(name, [128, 2048 // esz], dt))
            v = t[0:shape[0], 0:nel]
            if len(shape) == 3:
                v = v.rearrange("p (a b) -> p a b", b=shape[2])
            return Buf(v, name)

        identf = sb(es, "identf", [128, 128])
        identb = sb(es, "identb", [128, 128], BF16)
        ones_f = sb(es, "ones_f", [128, 128])
        triF = sb(es, "triF", [128, 128])
        triB = sb(es, "triB", [128, 128])
        sgtF = sb(es, "sgtF", [128, 128])
        sltB = sb(es, "sltB", [128, 128])
        cosT = sb(es, "cosT", [128, NT, 8])
        sinT = sb(es, "sinT", [128, NT, 8])

        def aff(buf, pattern_step, cmul, base, cmp):
            kk.op(pool, lambda: G.memset(buf[:], 1.0), w=[buf])
            kk.op(pool, lambda: G.affine_select(out=buf[:], in_=buf[:], pattern=[[pattern_step, 128]],
                                                compare_op=cmp, fill=0.0, base=base, channel_multiplier=cmul),
                  r=[buf], w=[buf])

        aff(identf, -1, 1, 0, ALU.is_equal)
        aff(triF, 1, -1, 0, ALU.is_ge)
        aff(triB, -1, 1, 0, ALU.is_ge)
        aff(sgtF, -1, 1, 0, ALU.is_gt)
        aff(sltB, 1, -1, 0, ALU.is_gt)
        kk.op(pool, lambda: G.memset(ones_f[:], 1.0), w=[ones_f])
        kk.op(dve, lambda: V.tensor_copy(out=identb[:], in_=identf[:]), r=[identf], w=[identb])

        with contextlib.ExitStack() as st:
            posi = sb(st, "posi", [128, NT], I32)
            posf = sb(st, "posf", [128, NT])
            invf = sb(st, "invf", [128, 8])
            ang = sb(st, "ang", [128, NT, 8])
            kf = sb(st, "kf", [128, NT, 8])
            ki = sb(st, "ki", [128, NT, 8], I32)
            rr = sb(st, "rr", [128, NT, 8])
            t2 = sb(st, "rt2", [128, NT, 8])
            kk.dma(sp, posi[:], pos_in[:, :], w=[posi])
            kk.dma(sp, invf[:], invf_in[:, :], w=[invf])
            kk.op(dve, lambda: V.tensor_copy(out=posf[:], in_=posi[:]), r=[posi], w=[posf])
            kk.op(dve, lambda: V.tensor_tensor(out=ang[:], in0=posf[:].unsqueeze(2).to_broadcast([128, NT, 8]),
                                               in1=invf[:].unsqueeze(1).to_broadcast([128, NT, 8]), op=ALU.mult),
                  r=[posf, invf], w=[ang])
            TWO_PI = float(2 * np.pi)
            C1 = 6.28125
            C2 = float(2 * np.pi - 6.28125)
            for (dst, shift) in ((sinT, 0.0), (cosT, float(np.pi / 2))):
                kk.op(dve, lambda: V.tensor_scalar(out=kf[:], in0=ang[:], scalar1=shift, scalar2=1.0 / TWO_PI,
                                                   op0=ALU.add, op1=ALU.mult), r=[ang], w=[kf])
                kk.op(dve, lambda: V.tensor_copy(out=ki[:], in_=kf[:]), r=[kf], w=[ki])
                kk.op(dve, lambda: V.tensor_copy(out=kf[:], in_=ki[:]), r=[ki], w=[kf])
                kk.op(dve, lambda: V.tensor_scalar(out=rr[:], in0=ang[:], scalar1=shift, scalar2=None, op0=ALU.add),
                      r=[ang], w=[rr])
                kk.op(dve, lambda: V.scalar_tensor_tensor(out=rr[:], in0=kf[:], scalar=-C1, in1=rr[:],
                                                          op0=ALU.mult, op1=ALU.add), r=[kf, rr], w=[rr])
                kk.op(dve, lambda: V.scalar_tensor_tensor(out=rr[:], in0=kf[:], scalar=-C2, in1=rr[:],
                                                          op0=ALU.mult, op1=ALU.add), r=[kf, rr], w=[rr])
                kk.op(dve, lambda: V.tensor_scalar(out=t2[:], in0=rr[:], scalar1=float(np.pi), scalar2=-TWO_PI,
                                                   op0=ALU.is_gt, op1=ALU.mult), r=[rr], w=[t2])
                kk.op(dve, lambda: V.tensor_tensor(out=rr[:], in0=rr[:], in1=t2[:], op=ALU.add), r=[rr, t2], w=[rr])
                kk.op(dve, lambda: V.tensor_scalar(out=t2[:], in0=rr[:], scalar1=float(-np.pi), scalar2=TWO_PI,
                                                   op0=ALU.is_lt, op1=ALU.mult), r=[rr], w=[t2])
                kk.op(dve, lambda: V.tensor_tensor(out=rr[:], in0=rr[:], in1=t2[:], op=ALU.add), r=[rr, t2], w=[rr])
                kk.op(dve, lambda: V.tensor_scalar(out=rr[:], in0=rr[:], scalar1=float(np.pi), scalar2=float(-np.pi),
                                                   op0=ALU.min, op1=ALU.max), r=[rr], w=[rr])
                kk.op(act, lambda: A.activation(out=dst[:], in_=rr[:], func=AF.Sin), r=[rr], w=[dst])
            kk.barrier()

        kk.dma(sp, xres[:, :], x_in[:, :])
        kk.barrier()

        def bc_load(q, buf, src_row_ap):
            kk.dma(q, buf[:], src_row_ap.partition_broadcast(128), w=[buf])

        def phase_mod(l):
            with contextlib.ExitStack() as st:
                cact = sb(st, "cact", [128, 8, NB])
                adab = sb(st, "adab", [1, 6 * D])
                modsb = sb(st, "modsb", [1, NB, 6 * D])
                wch = [sb(st, "adaw%d" % i, [128, 8, 512]) for i in range(2)]
                pm = [ps(st, "pmod%d" % i, [1, 512]) for i in range(2)]
                kk.dma(sp, cact[:], cT_in[:, :, :], w=[cact])
                kk.dma(sp, adab[:], ada_b[l:l + 1, :], w=[adab])
                kk.op(act, lambda: A.activation(out=cact[:], in_=cact[:], func=AF.Silu), r=[cact], w=[cact])
                for n in range(12):
                    wb = wch[n % 2]
                    kk.dma(sp, wb[:], ada_w[l, :, n * 512:(n + 1) * 512].rearrange("(k p) n -> p k n", p=128), w=[wb])
                    for b in range(NB):
                        pp = pm[(n * NB + b) % 2]
                        for k in range(8):
                            kk.op(pe, lambda: PE.matmul(pp[:], lhsT=cact[:, k, b:b + 1], rhs=wb[:, k, :],
                                                        start=(k == 0), stop=(k == 7)),
                                  r=[cact, wb], j=[pp])
                        kk.op(dve, lambda: V.tensor_tensor(out=modsb[0:1, b, n * 512:(n + 1) * 512], in0=pp[:],
                                                           in1=adab[0:1, n * 512:(n + 1) * 512], op=ALU.add),
                              r=[pp, adab], j=[modsb])
                for b in range(NB):
                    for idx in (1, 4):
                        kk.op(dve, lambda: V.tensor_scalar(out=modsb[0:1, b, idx * D:(idx + 1) * D],
                                                           in0=modsb[0:1, b, idx * D:(idx + 1) * D],
                                                           scalar1=1.0, scalar2=None, op0=ALU.add),
                              r=[modsb], w=[modsb])
                kk.dma(sp, modd[l:l + 1, :, :], modsb[:], r=[modsb])
                kk.barrier()

        def norm_mod_tile(st_bufs, i, l, gidx, hT, src):
            (xt, sq, ss, hb, G_bc, SH_bc, ptr) = st_bufs
            sq = sq[i % 2]
            ss = ss[i % 2]
            b = i // NTS
            x_t = xt[i % 2]
            h_b = hb[i % 2]
            p_t = ptr[i % 2]
            kk.dma(sp, x_t[:], src[i * 128:(i + 1) * 128, :], w=[x_t])
            kk.op(act, lambda: A.activation(out=sq[:], in_=x_t[:], func=AF.Square, accum_out=ss[:, 0:1]),
                  r=[x_t], w=[sq, ss])
            kk.op(act, lambda: A.activation(out=ss[:, 1:2], in_=ss[:, 0:1], func=AF.Sqrt, scale=1.0 / D, bias=epsb[:, 0:1]),
                  r=[ss, epsb], w=[ss])
            kk.op(dve, lambda: V.reciprocal(out=ss[:, 2:3], in_=ss[:, 1:2]), r=[ss], w=[ss])
            kk.op(dve, lambda: V.scalar_tensor_tensor(out=sq[:], in0=x_t[:], scalar=ss[:, 2:3], in1=G_bc[b][:],
                                                      op0=ALU.mult, op1=ALU.mult), r=[x_t, ss, G_bc[b]], w=[sq])
            kk.op(pool, lambda: G.tensor_tensor(out=h_b[:], in0=sq[:], in1=SH_bc[b][:], op=ALU.add),
                  r=[sq, SH_bc[b]], w=[h_b])
            for k in range(8):
                kk.op(pe, lambda: PE.transpose(out=p_t[:, k, :], in_=h_b[:, k * 128:(k + 1) * 128], identity=identb[:]),
                      r=[h_b, identb], j=[p_t])
            kk.op(act, lambda: A.copy(out=hT[:, :, i * 128:(i + 1) * 128], in_=p_t[:]), r=[p_t], j=[hT])
            return x_t, sq, h_b

        epsb = sb(es, "epsb", [128, 1])
        kk.op(pool, lambda: G.memset(epsb[:], EPS), w=[epsb])

        def load_mod_bc(st, l, gidx_scale, gidx_shift, norm_g, tag):
            Gs, SHs = [], []
            ng = sb(st, "ng" + tag, [128, D])
            bc_load(sp, ng, norm_g[l:l + 1, :])
            for b in range(NB):
                g_ = sb(st, "G%s%d" % (tag, b), [128, D])
                s_ = sb(st, "SH%s%d" % (tag, b), [128, D])
                bc_load(sp, g_, modd[l, b:b + 1, gidx_scale * D:(gidx_scale + 1) * D])
                bc_load(sp, s_, modd[l, b:b + 1, gidx_shift * D:(gidx_shift + 1) * D])
                kk.op(dve, lambda: V.tensor_tensor(out=g_[:], in0=g_[:], in1=ng[:], op=ALU.mult), r=[g_, ng], w=[g_])
                Gs.append(g_)
                SHs.append(s_)
            return Gs, SHs

        def phase_inproj(l):
            with contextlib.ExitStack() as st:
                hT = sb(st, "hT", [128, 8, T], BF16)
                with contextlib.ExitStack() as st1:
                    Gs, SHs = load_mod_bc(st1, l, 1, 0, norm1_g, "a")
                    xt = [sb(st1, "xt%d" % i, [128, D]) for i in range(2)]
                    sq = [sb(st1, "sq%d" % i, [128, D]) for i in range(2)]
                    ss = [sb(st1, "ss%d" % i, [128, 4]) for i in range(2)]
                    hb = [sb(st1, "hb%d" % i, [128, D], BF16) for i in range(2)]
                    ptr = [ps(st1, "ptr%d" % i, [128, 8, 128], BF16) for i in range(2)]
                    for i in range(NT):
                        norm_mod_tile((xt, sq, ss, hb, Gs, SHs, ptr), i, l, 0, hT, xres)
                    kk.barrier()
                with contextlib.ExitStack() as st2:
                    wb = [sb(st2, "winb%d" % i, [128, 8, 512], BF16) for i in range(2)]
                    stg = [sb(st2, "stg%d" % i, [128, 512]) for i in range(3)]
                    pp = [ps(st2, "ppj%d" % i, [128, 512]) for i in range(4)]
                    groups = [(c * 512, 512, c * 512) for c in range(7)] + [(ODT, 64, 3584)]
                    cnt = 0
                    for gi, (c0, w, d0) in enumerate(groups):
                        w_b = wb[gi % 2]
                        kk.dma(pool, w_b[:, :, 0:w], w_in[l, :, c0:c0 + w].rearrange("(k p) n -> p k n", p=128), w=[w_b])
                        for i in range(NT):
                            p_ = pp[cnt % 4]
                            s_ = stg[cnt % 3]
                            for k in range(8):
                                kk.op(pe, lambda: PE.matmul(p_[:, 0:w], lhsT=hT[:, k, i * 128:(i + 1) * 128],
                                                            rhs=w_b[:, k, 0:w], start=(k == 0), stop=(k == 7)),
                                      r=[hT, w_b], j=[p_])
                            if cnt % 2 == 0:
                                kk.op(act, lambda: A.copy(out=s_[:, 0:w], in_=p_[:, 0:w]), r=[p_], w=[s_])
                            else:
                                kk.op(dve, lambda: V.tensor_copy(out=s_[:, 0:w], in_=p_[:, 0:w]), r=[p_], w=[s_])
                            kk.dma(sp, proj_tm[i * 128:(i + 1) * 128, d0:d0 + w], s_[:, 0:w], r=[s_])
                            cnt += 1
                    kk.barrier()
                with contextlib.ExitStack() as st3:
                    wb = [sb(st3, "winc%d" % i, [128, 8, 512], BF16) for i in range(2)]
                    cw = sb(st3, "cw", [128, 24, 5])
                    cb = sb(st3, "cb", [128, 24])
                    xpad = [sb(st3, "xpad%d" % i, [128, S + 4]) for i in range(2)]
                    cv = [sb(st3, "cv%d" % i, [128, S]) for i in range(2)]
                    cvb = [sb(st3, "cvb%d" % i, [128, S], BF16) for i in range(2)]
                    tst = [sb(st3, "tst%d" % i, [128, 4, 128]) for i in range(2)]
                    tstb = [sb(st3, "tstb%d" % i, [128, 4, 128], BF16) for i in range(2)]
                    gsb = [sb(st3, "gsb%d" % i, [128, TG]) for i in range(2)]
                    pp = [ps(st3, "ppf%d" % i, [128, TG]) for i in range(3)]
                    ptf = [ps(st3, "ptf%d" % i, [128, 4, 128]) for i in range(2)]
                    ptb = [ps(st3, "ptb%d" % i, [128, 4, 128], BF16) for i in range(1)]
                    kk.dma(sp, cw[:], conv_w[l, :, :, :], w=[cw])
                    kk.dma(sp, cb[:], conv_b[l, :, :], w=[cb])
                    for xp in xpad:
                        kk.op(pool, lambda: G.memset(xp[:], 0.0), w=[xp])
                    cnt = 0
                    ccnt = 0
                    tcnt = 0
                    ncg = 6
                    for cg in range(ncg):
                        c0 = OXBC + cg * 512
                        w = min(512, IN_TOTAL - c0)
                        if c0 + w > ODT and c0 < OG:
                            pass
                        w_b = wb[cg % 2]
                        kk.dma(pool, w_b[:, :, 0:w], w_in[l, :, c0:c0 + w].rearrange("(k p) n -> p k n", p=128), w=[w_b])
                        for fc in range(w // 128 if w % 128 == 0 else (w + 127) // 128):
                            f0 = c0 + fc * 128
                            if f0 >= ODT:
                                continue
                            ch = (f0 - OXBC) // 128
                            for b in range(NB):
                                xp = xpad[ccnt % 2]
                                c_v = cv[ccnt % 2]
                                for tg in range(NTG):
                                    p_ = pp[cnt % 3]
                                    cnt += 1
                                    t0 = b * S + tg * TG
                                    for k in range(8):
                                        kk.op(pe, lambda: PE.matmul(p_[:], lhsT=w_b[:, k, fc * 128:(fc + 1) * 128],
                                                                    rhs=hT[:, k, t0:t0 + TG], start=(k == 0), stop=(k == 7)),
                                              r=[hT, w_b], j=[p_])
                                    kk.op(act, lambda: A.copy(out=xp[:, 2 + tg * TG:2 + (tg + 1) * TG], in_=p_[:]),
                                          r=[p_], j=[xp])
                                kk.op(dve, lambda: V.tensor_scalar(out=c_v[:], in0=xp[:, 0:S], scalar1=cw[:, ch, 0:1],
                                                                   scalar2=cb[:, ch:ch + 1], op0=ALU.mult, op1=ALU.add),
                                      r=[xp, cw, cb], w=[c_v])
                                for kq in range(1, 5):
                                    eng, E_ = (dve, V)
                                    kk.op(eng, lambda: E_.scalar_tensor_tensor(out=c_v[:], in0=xp[:, kq:kq + S],
                                                                               scalar=cw[:, ch, kq:kq + 1], in1=c_v[:],
                                                                               op0=ALU.mult, op1=ALU.add),
                                          r=[xp, cw, c_v], w=[c_v])
                                if ch < 16:
                                    kk.op(act, lambda: A.activation(out=c_v[:], in_=c_v[:], func=AF.Silu), r=[c_v], w=[c_v])
                                    for j0 in range(0, NTS, 4):
                                        nj = min(4, NTS - j0)
                                        p_t = ptf[tcnt % 2]
                                        t_s = tst[tcnt % 2]
                                        tcnt += 1
                                        for jj in range(nj):
                                            kk.op(pe, lambda: PE.transpose(out=p_t[:, jj, :],
                                                                           in_=c_v[:, (j0 + jj) * 128:(j0 + jj + 1) * 128],
                                                                           identity=identf[:]),
                                                  r=[c_v, identf], j=[p_t])
                                        kk.op(dve, lambda: V.tensor_copy(out=t_s[:, 0:nj, :], in_=p_t[:, 0:nj, :]),
                                              r=[p_t], w=[t_s])
                                        r0 = b * S + j0 * 128
                                        kk.dma(sp, xs_tm[r0:r0 + nj * 128, ch * 128:(ch + 1) * 128].rearrange("(j p) c -> p j c", p=128),
                                               t_s[:, 0:nj, :], r=[t_s])
                                else:
                                    c_b = cvb[ccnt % 2]
                                    kk.op(act, lambda: A.activation(out=c_b[:], in_=c_v[:], func=AF.Silu), r=[c_v], w=[c_b])
                                    gq = (ch - 16) % 4
                                    dst = bt_d if ch < 20 else ct_d
                                    kk.dma(sp, dst[gq, :, b * S:(b + 1) * S], c_b[:], r=[c_b])
                                    if ch < 20:
                                        for j0 in range(0, NTS, 4):
                                            nj = min(4, NTS - j0)
                                            p_t = ptb[0]
                                            t_s = tstb[tcnt % 2]
                                            tcnt += 1
                                            for jj in range(nj):
                                                kk.op(pe, lambda: PE.transpose(out=p_t[:, jj, :],
                                                                               in_=c_b[:, (j0 + jj) * 128:(j0 + jj + 1) * 128],
                                                                               identity=identb[:]),
                                                      r=[c_b, identb], j=[p_t])
                                            kk.op(dve, lambda: V.tensor_copy(out=t_s[:, 0:nj, :], in_=p_t[:, 0:nj, :]),
                                                  r=[p_t], w=[t_s])
                                            r0 = b * S + j0 * 128
                                            kk.dma(sp, btm_d[r0:r0 + nj * 128, gq * 128:(gq + 1) * 128].rearrange("(j p) c -> p j c", p=128),
                                                   t_s[:, 0:nj, :], r=[t_s])
                                ccnt += 1
                    for cg in range(4):
                        c0 = OG + cg * 512
                        w_b = wb[cg % 2]
                        kk.dma(pool, w_b[:], w_in[l, :, c0:c0 + 512].rearrange("(k p) n -> p k n", p=128), w=[w_b])
                        for fc in range(4):
                            gch = cg * 4 + fc
                            for b in range(NB):
                                for tg in range(NTG):
                                    p_ = pp[cnt % 3]
                                    g_s = gsb[cnt % 2]
                                    cnt += 1
                                    t0 = b * S + tg * TG
                                    for k in range(8):
                                        kk.op(pe, lambda: PE.matmul(p_[:], lhsT=w_b[:, k, fc * 128:(fc + 1) * 128],
                                                                    rhs=hT[:, k, t0:t0 + TG], start=(k == 0), stop=(k == 7)),
                                              r=[hT, w_b], j=[p_])
                                    kk.op(act, lambda: A.activation(out=g_s[:], in_=p_[:], func=AF.Sigmoid), r=[p_], w=[g_s])
                                    kk.dma(sp, gT_d[gch * 128:(gch + 1) * 128, t0:t0 + TG], g_s[:], r=[g_s])
                kk.barrier()

        def phase_attn(l, b, aoT):
            with contextlib.ExitStack() as st:
                qT = sb(st, "qT", [64, 16, S], BF16)
                kT = sb(st, "kT", [64, 4, S], BF16)
                vau = sb(st, "vau", [128, NTS, 4, 65], BF16)
                gq = sb(st, "gq", [128, 64])
                gk = sb(st, "gk", [128, 64])
                esk = sb(st, "esk", [128, 16])
                bc_load(sp, gq, q_norm_g[l:l + 1, :])
                bc_load(sp, gk, k_norm_g[l:l + 1, :])
                bc_load(sp, esk, attn_sink[l:l + 1, :])
                kk.op(act, lambda: A.activation(out=esk[:], in_=esk[:], func=AF.Exp), r=[esk], w=[esk])
                kk.op(pool, lambda: G.memset(vau[:], 1.0), w=[vau])
                with contextlib.ExitStack() as st1:
                    qkv = [sb(st1, "qkv%d" % i, [128, 1536]) for i in range(2)]
                    sq = sb(st1, "sqa", [128, 1280])
                    ssn = sb(st1, "ssn", [128, 20])
                    rstd = sb(st1, "rstd", [128, 20])
                    qkn = sb(st1, "qkn", [128, 1280])
                    qkr = [sb(st1, "qkr%d" % i, [128, 1280], BF16) for i in range(2)]
                    r1 = sb(st1, "r1", [128, 20, 8])
                    r2 = sb(st1, "r2", [128, 20, 8])
                    r3 = sb(st1, "r3", [128, 20, 8])
                    r4 = sb(st1, "r4", [128, 20, 8])
                    ptq = [ps(st1, "ptq%d" % i, [64, 8, 128], BF16) for i in range(4)]
                    ptk = [ps(st1, "ptk%d" % i, [64, 4, 128], BF16) for i in range(2)]
                    for j in range(NTS):
                        r0 = b * S + j * 128
                        q_ = qkv[j % 2]
                        q_r = qkr[j % 2]
                        kk.dma(sp, q_[:], proj_tm[r0:r0 + 128, 0:1536], w=[q_])
                        kk.op(dve, lambda: V.tensor_tensor(out=sq[:], in0=q_[:, 0:1280], in1=q_[:, 0:1280], op=ALU.mult),
                              r=[q_], w=[sq])
                        kk.op(dve, lambda: V.tensor_reduce(out=ssn[:], in_=sq[:].rearrange("p (h d) -> p h d", d=64),
                                                           axis=AX.X, op=ALU.add), r=[sq], w=[ssn])
                        kk.op(act, lambda: A.activation(out=rstd[:], in_=ssn[:], func=AF.Sqrt, scale=1.0 / 64, bias=epsb[:, 0:1]),
                              r=[ssn, epsb], w=[rstd])
                        kk.op(dve, lambda: V.reciprocal(out=rstd[:], in_=rstd[:]), r=[rstd], w=[rstd])
                        kk.op(dve, lambda: V.tensor_scalar(out=rstd[:, 0:16], in0=rstd[:, 0:16], scalar1=0.125, scalar2=None,
                                                           op0=ALU.mult), r=[rstd], w=[rstd])
                        q3 = q_[:, 0:1280].rearrange("p (h d) -> p h d", d=64)
                        n3 = qkn[:].rearrange("p (h d) -> p h d", d=64)
                        kk.op(dve, lambda: V.tensor_tensor(out=n3, in0=q3, in1=rstd[:].unsqueeze(2).to_broadcast([128, 20, 64]),
                                                           op=ALU.mult), r=[q_, rstd], w=[qkn])
                        kk.op(dve, lambda: V.tensor_tensor(out=n3[:, 0:16, :], in0=n3[:, 0:16, :],
                                                           in1=gq[:].unsqueeze(1).to_broadcast([128, 16, 64]), op=ALU.mult),
                              r=[qkn, gq], w=[qkn])
                        kk.op(dve, lambda: V.tensor_tensor(out=n3[:, 16:20, :], in0=n3[:, 16:20, :],
                                                           in1=gk[:].unsqueeze(1).to_broadcast([128, 4, 64]), op=ALU.mult),
                              r=[qkn, gk], w=[qkn])
                        tix = b * NTS + j
                        cb_ = cosT[:, tix, :].unsqueeze(1).to_broadcast([128, 20, 8])
                        sb_ = sinT[:, tix, :].unsqueeze(1).to_broadcast([128, 20, 8])
                        t1_ = n3[:, :, 0:8]
                        t2_ = n3[:, :, 8:16]
                        kk.op(dve, lambda: V.tensor_tensor(out=r1[:], in0=t1_, in1=cb_, op=ALU.mult), r=[qkn, cosT], w=[r1])
                        kk.op(pool, lambda: G.tensor_tensor(out=r2[:], in0=t2_, in1=sb_, op=ALU.mult), r=[qkn, sinT], w=[r2])
                        kk.op(dve, lambda: V.tensor_tensor(out=r3[:], in0=t2_, in1=cb_, op=ALU.mult), r=[qkn, cosT], w=[r3])
                        kk.op(pool, lambda: G.tensor_tensor(out=r4[:], in0=t1_, in1=sb_, op=ALU.mult), r=[qkn, sinT], w=[r4])
                        kk.op(act, lambda: A.copy(out=q_r[:], in_=qkn[:]), r=[qkn], w=[q_r])
                        qr3 = q_r[:].rearrange("p (h d) -> p h d", d=64)
                        kk.op(dve, lambda: V.tensor_tensor(out=qr3[:, :, 0:8], in0=r1[:], in1=r2[:], op=ALU.subtract),
                              r=[r1, r2, q_r], w=[q_r])
                        kk.op(dve, lambda: V.tensor_tensor(out=qr3[:, :, 8:16], in0=r3[:], in1=r4[:], op=ALU.add),
                              r=[r3, r4, q_r], w=[q_r])
                        pq0 = ptq[(2 * j) % 4]
                        pq1 = ptq[(2 * j + 1) % 4]
                        pk = ptk[j % 2]
                        for h in range(20):
                            dst = pq0 if h < 8 else (pq1 if h < 16 else pk)
                            hh = h % 8 if h < 16 else h - 16
                            kk.op(pe, lambda: PE.transpose(out=dst[:, hh, :], in_=qr3[:, h, :], identity=identb[:]),
                                  r=[q_r, identb], j=[dst])
                        kk.op(act, lambda: A.copy(out=qT[:, 0:8, j * 128:(j + 1) * 128], in_=pq0[:]), r=[pq0], j=[qT])
                        kk.op(dve, lambda: V.tensor_copy(out=qT[:, 8:16, j * 128:(j + 1) * 128], in_=pq1[:]), r=[pq1], j=[qT])
                        kk.op(act, lambda: A.copy(out=kT[:, :, j * 128:(j + 1) * 128], in_=pk[:]), r=[pk], j=[kT])
                        kk.op(pool, lambda: G.tensor_copy(out=vau[:, j, :, 0:64],
                                                          in_=q_[:, 1280:1536].rearrange("p (g d) -> p g d", d=64)),
                              r=[q_], j=[vau])
                    kk.barrier()
                with contextlib.ExitStack() as st2:
                    sc = [ps(st2, "sc%d" % i, [128, 512]) for i in range(4)]
                    ops_ = [ps(st2, "ops%d" % i, [128, 4, 65]) for i in range(2)]
                    pta = ps(st2, "pta", [128, 8, 128], BF16)
                    pT = [sb(st2, "pT%d" % i, [128, 512], BF16) for i in range(6)]
                    den = sb(st2, "den", [128, 4])
                    ao = [sb(st2, "ao%d" % i, [128, 1024], BF16) for i in range(2)]
                    cnt = 0
                    gcnt = 0
                    for j in range(NTS):
                        a_o = ao[j % 2]
                        ao3 = a_o[:].rearrange("p (h d) -> p h d", d=64)
                        for g in range(4):
                            jjs = [jj for jj in (j - 1, j, j + 1) if 0 <= jj < NTS]
                            pts = []
                            for jj in jjs:
                                s_ = sc[cnt % 4]
                                p_ = pT[cnt % 6]
                                cnt += 1
                                kk.op(pe, lambda: PE.matmul(s_[:], lhsT=kT[:, g, jj * 128:(jj + 1) * 128],
                                                            rhs=qT[:, 4 * g:4 * g + 4, j * 128:(j + 1) * 128],
                                                            start=True, stop=True), r=[kT, qT], w=[s_])
                                kk.op(act, lambda: A.activation(out=p_[:], in_=s_[:], func=AF.Exp), r=[s_], w=[p_])
                                if jj != j:
                                    mk = triB if jj < j else triF
                                    p3 = p_[:].rearrange("p (h q) -> p h q", q=128)
                                    kk.op(dve, lambda: V.tensor_tensor(out=p3, in0=p3,
                                                                       in1=mk[:].unsqueeze(1).to_broadcast([128, 4, 128]),
                                                                       op=ALU.mult), r=[p_, mk], w=[p_])
                                pts.append((jj, p_))
                            o_ = ops_[gcnt % 2]
                            gcnt += 1
                            for h in range(4):
                                for ii, (jj, p_) in enumerate(pts):
                                    kk.op(pe, lambda: PE.matmul(o_[:, h, :], lhsT=p_[:, h * 128:(h + 1) * 128],
                                                                rhs=vau[:, jj, g, :], start=(ii == 0), stop=(ii == len(pts) - 1)),
                                          r=[p_, vau], j=[o_])
                            kk.op(dve, lambda: V.tensor_tensor(out=den[:], in0=o_[:, :, 64], in1=esk[:, 4 * g:4 * g + 4], op=ALU.add),
                                  r=[o_, esk], w=[den])
                            kk.op(dve, lambda: V.reciprocal(out=den[:], in_=den[:]), r=[den], w=[den])
                            kk.op(dve, lambda: V.tensor_tensor(out=ao3[:, 4 * g:4 * g + 4, :], in0=o_[:, :, 0:64],
                                                               in1=den[:].unsqueeze(2).to_broadcast([128, 4, 64]), op=ALU.mult),
                                  r=[o_, den], j=[a_o])
                        for k in range(8):
                            kk.op(pe, lambda: PE.transpose(out=pta[:, k, :], in_=a_o[:, k * 128:(k + 1) * 128], identity=identb[:]),
                                  r=[a_o, identb], j=[pta])
                        kk.op(act, lambda: A.copy(out=aoT[:, :, j * 128:(j + 1) * 128], in_=pta[:]), r=[pta], j=[aoT])
                    kk.barrier()

        def phase_ssd(l, b, ysT):
            with contextlib.ExitStack() as st:
                dtb = sb(st, "dtb", [128, 64])
                aneg = sb(st, "aneg", [128, 64])
                dsk = sb(st, "dsk", [128, 32])
                ngs = sb(st, "ngs", [128, 2048])
                bc_load(sp, dtb, dt_bias[l:l + 1, :])
                bc_load(sp, aneg, a_log[l:l + 1, :])
                bc_load(sp, dsk, ssm_d[l:l + 1, :])
                bc_load(sp, ngs, ssm_norm_g[l:l + 1, :])
                kk.op(act, lambda: A.activation(out=aneg[:], in_=aneg[:], func=AF.Exp), r=[aneg], w=[aneg])
                kk.op(dve, lambda: V.tensor_scalar(out=aneg[:], in0=aneg[:], scalar1=-1.0, scalar2=None, op0=ALU.mult),
                      r=[aneg], w=[aneg])
                xs = sb(st, "xs", [128, 2048])
                dtr = sb(st, "dtr", [128, 64])
                dt_ = sb(st, "dt", [128, 64])
                da = sb(st, "da", [128, 64])
                cpsb = sb(st, "cpsb", [128, 128])
                ecum = sb(st, "ecum", [128, 64])
                edte = sb(st, "edte", [128, 64])
                etot = sb(st, "etot", [128, 64])
                w2 = sb(st, "w2", [128, 64])
                xdt = [sb(st, "xdt%d" % d, [128, 2048], BF16) for d in range(2)]
                xdte = [sb(st, "xdte%d" % d, [128, 2048], BF16) for d in range(2)]
                Hs = [sb(st, "H%d" % d, [128, 2048]) for d in range(2)]
                Hb = [sb(st, "Hb%d" % d, [128, 2048], BF16) for d in range(2)]
                btc = sb(st, "btc", [128, 4, 128], BF16)
                ctc = sb(st, "ctc", [128, 4, 128], BF16)
                btm = sb(st, "btm", [128, 512], BF16)
                cps = ps(st, "cps", [128, 128])
                big = [ps(st, "big%d" % i, [128, 512]) for i in range(4)]
                bigc = [0]

                def nbig():
                    bigc[0] += 1
                    return big[bigc[0] % 4]

                def prep(c, dirs):
                    r0 = b * S + c * 128
                    kk.dma(sp, xs[:], xs_tm[r0:r0 + 128, :], w=[xs])
                    kk.dma(sp, dtr[:], proj_tm[r0:r0 + 128, 3584:3648], w=[dtr])
                    kk.dma(sp, btm[:], btm_d[r0:r0 + 128, :], w=[btm])
                    kk.op(dve, lambda: V.tensor_tensor(out=dt_[:], in0=dtr[:], in1=dtb[:], op=ALU.add), r=[dtr, dtb], w=[dt_])
                    kk.op(act, lambda: A.activation(out=dt_[:], in_=dt_[:], func=AF.Exp), r=[dt_], w=[dt_])
                    kk.op(act, lambda: A.activation(out=dt_[:], in_=dt_[:], func=AF.Ln, bias=oneb[:, 0:1]), r=[dt_, oneb], w=[dt_])
                    kk.op(dve, lambda: V.tensor_tensor(out=da[:], in0=dt_[:], in1=aneg[:], op=ALU.mult), r=[dt_, aneg], w=[da])
                    if XP == 1:
                        return
                    kk.op(pe, lambda: PE.matmul(cps[:, 0:32], lhsT=triF[:], rhs=da[:, 0:32], start=True, stop=True),
                          r=[triF, da], w=[cps])
                    kk.op(pe, lambda: PE.matmul(cps[:, 32:64], lhsT=triB[:], rhs=da[:, 32:64], start=True, stop=True),
                          r=[triB, da], j=[cps])
                    kk.op(pe, lambda: PE.matmul(cps[:, 64:128], lhsT=ones_f[:], rhs=da[:], start=True, stop=True),
                          r=[ones_f, da], j=[cps])
                    if XP == 3:
                        return
                    kk.op(dve, lambda: V.tensor_copy(out=cpsb[:], in_=cps[:]), r=[cps], w=[cpsb])
                    kk.op(act, lambda: A.activation(out=ecum[:], in_=cpsb[:, 0:64], func=AF.Exp), r=[cpsb], w=[ecum])
                    kk.op(act, lambda: A.activation(out=etot[:], in_=cpsb[:, 64:128], func=AF.Exp), r=[cpsb], w=[etot])
                    kk.op(dve, lambda: V.tensor_tensor(out=edte[:], in0=cpsb[:, 64:128], in1=cpsb[:, 0:64], op=ALU.subtract),
                          r=[cpsb], w=[edte])
                    kk.op(act, lambda: A.activation(out=edte[:], in_=edte[:], func=AF.Exp), r=[edte], w=[edte])
                    kk.op(dve, lambda: V.tensor_tensor(out=w2[:], in0=dt_[:], in1=edte[:], op=ALU.mult), r=[dt_, edte], w=[w2])
                    if XP == 4:
                        return
                    x3 = xs[:].rearrange("p (h d) -> p h d", d=64)
                    main = len(dirs) == 2
                    for d in (dirs if main else []):
                        kk.op(pool, lambda: G.tensor_tensor(out=xdt[d][:].rearrange("p (h d) -> p h d", d=64), in0=x3,
                                                            in1=dt_[:, d * 32:(d + 1) * 32].unsqueeze(2).to_broadcast([128, 32, 64]),
                                                            op=ALU.mult), r=[xs, dt_], w=[xdt[d]])
                    for d in ([0] if main else [1]):
                        kk.op(pool, lambda: G.tensor_tensor(out=xdte[d][:].rearrange("p (h d) -> p h d", d=64), in0=x3,
                                                            in1=w2[:, d * 32:(d + 1) * 32].unsqueeze(2).to_broadcast([128, 32, 64]),
                                                            op=ALU.mult), r=[xs, w2], w=[xdte[d]])

                def state_update(c, d):
                    H = Hs[d]
                    kk.op(dve, lambda: V.tensor_tensor(out=H[:].rearrange("p (h d) -> p h d", d=64),
                                                       in0=H[:].rearrange("p (h d) -> p h d", d=64),
                                                       in1=etot[:, d * 32:(d + 1) * 32].unsqueeze(2).to_broadcast([128, 32, 64]),
                                                       op=ALU.mult), r=[H, etot], w=[H])
                    for g in range(4):
                        sp_ = nbig()
                        kk.op(pe, lambda: PE.matmul(sp_[:], lhsT=btm[:, g * 128:(g + 1) * 128],
                                                    rhs=xdte[d][:, g * 512:(g + 1) * 512], start=True, stop=True),
                              r=[btm, xdte[d]], w=[sp_])
                        kk.op(dve, lambda: V.tensor_tensor(out=H[:, g * 512:(g + 1) * 512], in0=H[:, g * 512:(g + 1) * 512],
                                                           in1=sp_[:], op=ALU.add), r=[H, sp_], w=[H])

                kk.op(pool, lambda: G.memset(Hs[1][:], 0.0), w=[Hs[1]])
                for c in range(NTS - 1, -1, -1):
                    kk.op(act, lambda: A.copy(out=Hb[1][:], in_=Hs[1][:]), r=[Hs[1]], w=[Hb[1]])
                    kk.dma(sp, gst_d[b, c, :, :], Hb[1][:], r=[Hb[1]])
                    if c > 0 and XP >= 1:
                        prep(c, [1])
                        if XP == 2:
                            state_update(c, 1)
                kk.barrier()
                if stop == "ssd_pre":
                    return

                with contextlib.ExitStack() as st2:
                    cbp = ps(st2, "cbp", [128, 4, 128])
                    yps = ps(st2, "yps", [128, 512])
                    pty = ps(st2, "pty", [128, 8, 128], BF16)
                    cbm = [sb(st2, "cbm%d" % d, [128, 4, 128]) for d in range(2)]
                    rhsD = [sb(st2, "rhsD%d" % d, [128, 8, 128]) for d in range(2)]
                    ex = [sb(st2, "ex%d" % i, [128, 512]) for i in range(2)]
                    MT = [sb(st2, "MT%d" % d, [128, 8, 128], BF16) for d in range(2)]
                    yy = sb(st2, "yy", [128, 2048])
                    zz = sb(st2, "zz", [128, 2048])
                    tA = sb(st2, "tA", [128, 512])
                    tB = sb(st2, "tB", [128, 512])
                    tC = sb(st2, "tC", [128, 512])
                    ssy = sb(st2, "ssy", [128, 4])
                    ynb = xdt[0]
                    kk.op(pool, lambda: G.memset(Hs[0][:], 0.0), w=[Hs[0]])
                    kk.op(pool, lambda: G.memset(Hb[0][:], 0.0), w=[Hb[0]])
                    Us = [sgtF, sltB]
                    Ts = [triF, triB]
                    exc = 0
                    for c in range(NTS):
                        r0 = b * S + c * 128
                        cs = slice(r0, r0 + 128)
                        prep(c, [0, 1])
                        kk.dma(sp, Hb[1][:], gst_d[b, c, :, :], w=[Hb[1]])
                        kk.dma(sp, zz[:], proj_tm[r0:r0 + 128, 1536:3584], w=[zz])
                        for g in range(4):
                            kk.dma(sp, btc[:, g, :], bt_d[g, :, cs], j=[btc])
                            kk.dma(sp, ctc[:, g, :], ct_d[g, :, cs], j=[ctc])
                        for g in range(4):
                            kk.op(pe, lambda: PE.matmul(cbp[:, g, :], lhsT=btc[:, g, :], rhs=ctc[:, g, :], start=True, stop=True),
                                  r=[btc, ctc], j=[cbp])
                        for d in range(2):
                            kk.op(dve, lambda: V.tensor_tensor(out=cbm[d][:], in0=cbp[:],
                                                               in1=Ts[d][:].unsqueeze(1).to_broadcast([128, 4, 128]), op=ALU.mult),
                                  r=[cbp, Ts[d]], w=[cbm[d]])
                        for g in range(4):
                            for d in range(2):
                                kk.op(pool, lambda: G.tensor_tensor(out=rhsD[d][:],
                                                                    in0=Ts[d][:].unsqueeze(1).to_broadcast([128, 8, 128]),
                                                                    in1=da[:, d * 32 + g * 8:d * 32 + g * 8 + 8].unsqueeze(2).to_broadcast([128, 8, 128]),
                                                                    op=ALU.mult), r=[Ts[d], da], w=[rhsD[d]])
                                for hf in range(2):
                                    dp = nbig()
                                    e_ = ex[exc % 2]
                                    exc += 1
                                    kk.op(pe, lambda: PE.matmul(dp[:], lhsT=Us[d][:], rhs=rhsD[d][:, hf * 4:hf * 4 + 4, :],
                                                                start=True, stop=True), r=[Us[d], rhsD[d]], w=[dp])
                                    kk.op(act, lambda: A.activation(out=e_[:], in_=dp[:], func=AF.Exp), r=[dp], w=[e_])
                                    kk.op(dve, lambda: V.tensor_tensor(out=MT[d][:, hf * 4:hf * 4 + 4, :],
                                                                       in0=e_[:].rearrange("p (h l) -> p h l", l=128),
                                                                       in1=cbm[d][:, g, :].unsqueeze(1).to_broadcast([128, 4, 128]),
                                                                       op=ALU.mult), r=[e_, cbm[d]], j=[MT[d]])
                            for h8 in range(8):
                                h = g * 8 + h8
                                for d in range(2):
                                    kk.op(pe, lambda: PE.matmul(yps[:, h8 * 64:(h8 + 1) * 64], lhsT=MT[d][:, h8, :],
                                                                rhs=xdt[d][:, h * 64:(h + 1) * 64], start=(d == 0), stop=(d == 1)),
                                          r=[MT[d], xdt[d]], j=[yps])
                            yo = [nbig(), nbig()]
                            for d in range(2):
                                kk.op(pe, lambda: PE.matmul(yo[d][:], lhsT=ctc[:, g, :], rhs=Hb[d][:, g * 512:(g + 1) * 512],
                                                            start=True, stop=True), r=[ctc, Hb[d]], w=[yo[d]])
                            gs = slice(g * 512, (g + 1) * 512)

                            def bc8(t, d):
                                return t[:, d * 32 + g * 8:d * 32 + g * 8 + 8].unsqueeze(2).to_broadcast([128, 8, 64])
                            v3 = lambda t: t[:].rearrange("p (h d) -> p h d", d=64)
                            kk.op(dve, lambda: V.tensor_tensor(out=v3(tA), in0=v3(yo[0]), in1=bc8(ecum, 0), op=ALU.mult),
                                  r=[yo[0], ecum], w=[tA])
                            kk.op(dve, lambda: V.tensor_tensor(out=v3(tB), in0=v3(yo[1]), in1=bc8(ecum, 1), op=ALU.mult),
                                  r=[yo[1], ecum], w=[tB])
                            kk.op(pool, lambda: G.tensor_tensor(out=v3(tC), in0=xs[:, gs].rearrange("p (h d) -> p h d", d=64),
                                                                in1=dsk[:, g * 8:g * 8 + 8].unsqueeze(2).to_broadcast([128, 8, 64]),
                                                                op=ALU.mult), r=[xs, dsk], w=[tC])
                            kk.op(dve, lambda: V.tensor_tensor(out=tA[:], in0=tA[:], in1=yps[:], op=ALU.add), r=[tA, yps], w=[tA])
                            kk.op(pool, lambda: G.tensor_tensor(out=tB[:], in0=tB[:], in1=tC[:], op=ALU.add), r=[tB, tC], w=[tB])
                            kk.op(dve, lambda: V.tensor_tensor(out=yy[:, gs], in0=tA[:], in1=tB[:], op=ALU.add), r=[tA, tB], j=[yy])
                        if stop == "ssd_nofin":
                            continue
                        state_update(c, 0)
                        kk.op(act, lambda: A.copy(out=Hb[0][:], in_=Hs[0][:]), r=[Hs[0]], w=[Hb[0]])
                        kk.op(act, lambda: A.activation(out=zz[:], in_=zz[:], func=AF.Silu), r=[zz], w=[zz])
                        kk.op(dve, lambda: V.tensor_tensor(out=yy[:], in0=yy[:], in1=zz[:], op=ALU.mult), r=[yy, zz], w=[yy])
                        for g in range(4):
                            kk.op(act, lambda: A.activation(out=zz[:, g * 512:(g + 1) * 512], in_=yy[:, g * 512:(g + 1) * 512],
                                                            func=AF.Square, accum_out=ssy[:, g:g + 1]), r=[yy], w=[zz, ssy] if g == 0 else (), j=() if g == 0 else [zz, ssy])
                        kk.op(act, lambda: A.activation(out=ssy[:], in_=ssy[:], func=AF.Sqrt, scale=1.0 / 512, bias=epsb[:, 0:1]),
                              r=[ssy, epsb], w=[ssy])
                        kk.op(dve, lambda: V.reciprocal(out=ssy[:], in_=ssy[:]), r=[ssy], w=[ssy])
                        kk.op(dve, lambda: V.tensor_tensor(out=yy[:].rearrange("p (g d) -> p g d", d=512),
                                                           in0=yy[:].rearrange("p (g d) -> p g d", d=512),
                                                           in1=ssy[:].unsqueeze(2).to_broadcast([128, 4, 512]), op=ALU.mult),
                              r=[yy, ssy], w=[yy])
                        kk.op(pool, lambda: G.tensor_tensor(out=ynb[:], in0=yy[:], in1=ngs[:], op=ALU.mult), r=[yy, ngs], w=[ynb])
                        for half in range(2):
                            for k in range(8):
                                kc = half * 8 + k
                                kk.op(pe, lambda: PE.transpose(out=pty[:, k, :], in_=ynb[:, kc * 128:(kc + 1) * 128], identity=identb[:]),
                                      r=[ynb, identb], j=[pty])
                            kk.op(act, lambda: A.copy(out=ysT[:, half * 8:half * 8 + 8, c * 128:(c + 1) * 128], in_=pty[:]),
                                  r=[pty], j=[ysT])
                    kk.barrier()

        def phase_outproj(l, b, aoT, ysT):
            with contextlib.ExitStack() as st:
                wao = sb(st, "wao", [128, 8, D], BF16)
                wso = sb(st, "wso", [128, 16, D], BF16)
                wo = sb(st, "wo", [128, 8, D], BF16)
                g1 = sb(st, "g1bc", [128, D])
                kk.dma(pool, wao[:], w_attn_o[l, :, :].rearrange("(k p) n -> p k n", p=128), w=[wao])
                kk.dma(pool, wso[:], w_ssm_o[l, :, :].rearrange("(k p) n -> p k n", p=128), w=[wso])
                kk.dma(pool, wo[:], w_out[l, :, :].rearrange("(k p) n -> p k n", p=128), w=[wo])
                bc_load(sp, g1, modd[l, b:b + 1, 2 * D:3 * D])
                mT = sb(st, "mT", [128, 8, TG], BF16)
                gA = [sb(st, "gA%d" % i, [128, TG]) for i in range(2)]
                gS = [sb(st, "gS%d" % i, [128, TG]) for i in range(2)]
                t1 = [sb(st, "mt1%d" % i, [128, TG]) for i in range(2)]
                t2 = [sb(st, "mt2%d" % i, [128, TG]) for i in range(2)]
                xt = [sb(st, "oxt%d" % i, [128, D]) for i in range(1)]
                xn = [sb(st, "oxn%d" % i, [128, D]) for i in range(1)]
                tmp = sb(st, "otmp", [128, 512])
                pa = [ps(st, "pa%d" % i, [128, TG]) for i in range(2)]
                pss = [ps(st, "pss%d" % i, [128, TG]) for i in range(2)]
                po = [ps(st, "po%d" % i, [128, 512]) for i in range(2)]
                mc = 0
                tc_ = 0
                for tg in range(NTG):
                    tl = tg * TG
                    t0 = b * S + tl
                    for m in range(8):
                        p_a = pa[mc % 2]
                        p_s = pss[mc % 2]
                        g_a = gA[mc % 2]
                        g_s = gS[mc % 2]
                        t_1 = t1[mc % 2]
                        t_2 = t2[mc % 2]
                        mc += 1
                        kk.dma(sp, g_a[:], gT_d[m * 128:(m + 1) * 128, t0:t0 + TG], w=[g_a])
                        kk.dma(sp, g_s[:], gT_d[1024 + m * 128:1024 + (m + 1) * 128, t0:t0 + TG], w=[g_s])
                        for k in range(8):
                            kk.op(pe, lambda: PE.matmul(p_a[:], lhsT=wao[:, k, m * 128:(m + 1) * 128], rhs=aoT[:, k, tl:tl + TG],
                                                        start=(k == 0), stop=(k == 7)), r=[wao, aoT], j=[p_a])
                        for k in range(16):
                            kk.op(pe, lambda: PE.matmul(p_s[:], lhsT=wso[:, k, m * 128:(m + 1) * 128], rhs=ysT[:, k, tl:tl + TG],
                                                        start=(k == 0), stop=(k == 15)), r=[wso, ysT], j=[p_s])
                        kk.op(dve, lambda: V.tensor_tensor(out=t_1[:], in0=p_a[:], in1=g_a[:], op=ALU.mult), r=[p_a, g_a], w=[t_1])
                        kk.op(dve, lambda: V.tensor_tensor(out=t_2[:], in0=p_s[:], in1=g_s[:], op=ALU.mult), r=[p_s, g_s], w=[t_2])
                        kk.op(pool, lambda: G.tensor_tensor(out=mT[:, m, :], in0=t_1[:], in1=t_2[:], op=ALU.add),
                              r=[t_1, t_2], j=[mT])
                    for i in range(TG // 128):
                        r0 = t0 + i * 128
                        x_t = xt[0]
                        x_n = xn[0]
                        tc_ += 1
                        kk.dma(sp, x_t[:], xres[r0:r0 + 128, :], w=[x_t])
                        for n in range(2):
                            p_o = po[n]
                            for m in range(8):
                                kk.op(pe, lambda: PE.matmul(p_o[:], lhsT=mT[:, m, i * 128:(i + 1) * 128], rhs=wo[:, m, n * 512:(n + 1) * 512],
                                                            start=(m == 0), stop=(m == 7)), r=[mT, wo], j=[p_o])
                            kk.op(dve, lambda: V.tensor_tensor(out=tmp[:], in0=p_o[:], in1=g1[:, n * 512:(n + 1) * 512], op=ALU.mult),
                                  r=[p_o, g1], w=[tmp])
                            kk.op(pool, lambda: G.tensor_tensor(out=x_n[:, n * 512:(n + 1) * 512], in0=tmp[:],
                                                                in1=x_t[:, n * 512:(n + 1) * 512], op=ALU.add),
                                  r=[tmp, x_t], j=[x_n])
                        kk.dma(sp, xres[r0:r0 + 128, :], x_n[:], r=[x_n])
                kk.barrier()

        def phase_moe(l, final):
            dst = y_out if final else xres
            for b in range(NB):
                with contextlib.ExitStack() as st:
                    h2T = sb(st, "h2T", [128, 8, S], BF16)
                    comb = sb(st, "comb", [128, NTS, NE])
                    accs = [sb(st, "acc%d" % j, [128, D]) for j in range(NTS)]
                    bg = sb(st, "bg", [128, NE, 8])
                    bu = sb(st, "bu", [128, NE, 8])
                    kk.dma(sp, bg[:], b_gate[l, :, :, :], w=[bg])
                    kk.dma(sp, bu[:], b_up[l, :, :, :], w=[bu])
                    kk.op(dve, lambda: V.tensor_scalar(out=bu[:], in0=bu[:], scalar1=1.0, scalar2=None, op0=ALU.add), r=[bu], w=[bu])
                    with contextlib.ExitStack() as st1:
                        ng = sb(st1, "ng2", [128, D])
                        G2 = sb(st1, "G2", [128, D])
                        SH2 = sb(st1, "SH2", [128, D])
                        bc_load(sp, ng, norm2_g[l:l + 1, :])
                        bc_load(sp, G2, modd[l, b:b + 1, 4 * D:5 * D])
                        bc_load(sp, SH2, modd[l, b:b + 1, 3 * D:4 * D])
                        kk.op(dve, lambda: V.tensor_tensor(out=G2[:], in0=G2[:], in1=ng[:], op=ALU.mult), r=[G2, ng], w=[G2])
                        rw = sb(st1, "rw", [128, 8, NE])
                        rb = sb(st1, "rb", [128, NE])
                        bd = sb(st1, "bd", [NE, D])
                        kk.dma(sp, rw[:], router_w[l, :, :].rearrange("(k p) e -> p k e", p=128), w=[rw])
                        bc_load(sp, rb, router_b[l:l + 1, :])
                        kk.dma(sp, bd[:], b_down[l, :, :], w=[bd])
                        xt = [sb(st1, "mxt%d" % i, [128, D]) for i in range(2)]
                        hf = [sb(st1, "mhf%d" % i, [128, D]) for i in range(2)]
                        hb = [sb(st1, "mhb%d" % i, [128, D], BF16) for i in range(2)]
                        hTf2 = [sb(st1, "hTf%d" % i, [128, 8, 128]) for i in range(2)]
                        lg2 = [sb(st1, "lg%d" % i, [128, NE]) for i in range(2)]
                        mx82 = [sb(st1, "mx8%d" % i, [128, 8]) for i in range(2)]
                        msk2 = [sb(st1, "msk%d" % i, [128, NE]) for i in range(2)]
                        nmx2 = [sb(st1, "nmx%d" % i, [128, 1]) for i in range(2)]
                        ee2 = [sb(st1, "ee%d" % i, [128, NE]) for i in range(2)]
                        dn2 = [sb(st1, "dn%d" % i, [128, 1]) for i in range(2)]
                        cT2 = [sb(st1, "cT%d" % i, [NE, 128]) for i in range(2)]
                        sq2 = [sb(st1, "msq%d" % i, [128, D]) for i in range(2)]
                        ss2 = [sb(st1, "mss%d" % i, [128, 4]) for i in range(2)]
                        ptr = [ps(st1, "mptr%d" % i, [128, 8, 128], BF16) for i in range(2)]
                        ptf = [ps(st1, "mptf%d" % i, [128, 4, 128]) for i in range(2)]
                        plg = ps(st1, "plg", [128, NE])
                        pct = ps(st1, "pct", [NE, 128])
                        pbd = [ps(st1, "pbd%d" % i, [128, 512]) for i in range(2)]
                        for j in range(NTS):
                            r0 = b * S + j * 128
                            x_t = xt[j % 2]
                            h_f = hf[j % 2]
                            h_b = hb[j % 2]
                            p_t = ptr[j % 2]
                            hTf, lg, mx8, msk, nmx, ee, dn, cT, sq, ss = (hTf2[j % 2], lg2[j % 2], mx82[j % 2], msk2[j % 2], nmx2[j % 2],
                                                                         ee2[j % 2], dn2[j % 2], cT2[j % 2], sq2[j % 2], ss2[j % 2])
                            kk.dma(sp, x_t[:], xres[r0:r0 + 128, :], w=[x_t])
                            kk.op(act, lambda: A.activation(out=sq[:], in_=x_t[:], func=AF.Square, accum_out=ss[:, 0:1]),
                                  r=[x_t], w=[sq, ss])
                            kk.op(act, lambda: A.activation(out=ss[:, 1:2], in_=ss[:, 0:1], func=AF.Sqrt, scale=1.0 / D, bias=epsb[:, 0:1]),
                                  r=[ss, epsb], w=[ss])
                            kk.op(dve, lambda: V.reciprocal(out=ss[:, 2:3], in_=ss[:, 1:2]), r=[ss], w=[ss])
                            kk.op(dve, lambda: V.scalar_tensor_tensor(out=sq[:], in0=x_t[:], scalar=ss[:, 2:3], in1=G2[:],
                                                                      op0=ALU.mult, op1=ALU.mult), r=[x_t, ss, G2], w=[sq])
                            kk.op(pool, lambda: G.tensor_tensor(out=h_f[:], in0=sq[:], in1=SH2[:], op=ALU.add), r=[sq, SH2], w=[h_f])
                            kk.op(act, lambda: A.copy(out=h_b[:], in_=h_f[:]), r=[h_f], w=[h_b])
                            for k in range(8):
                                kk.op(pe, lambda: PE.transpose(out=p_t[:, k, :], in_=h_b[:, k * 128:(k + 1) * 128], identity=identb[:]),
                                      r=[h_b, identb], j=[p_t])
                            kk.op(act, lambda: A.copy(out=h2T[:, :, j * 128:(j + 1) * 128], in_=p_t[:]), r=[p_t], j=[h2T])
                            for half in range(2):
                                pf = ptf[half]
                                for k in range(4):
                                    kc = half * 4 + k
                                    kk.op(pe, lambda: PE.transpose(out=pf[:, k, :], in_=h_f[:, kc * 128:(kc + 1) * 128], identity=identf[:]),
                                          r=[h_f, identf], j=[pf])
                                kk.op(dve, lambda: V.tensor_copy(out=hTf[:, half * 4:half * 4 + 4, :], in_=pf[:]), r=[pf], j=[hTf])
                            for k in range(8):
                                kk.op(pe, lambda: PE.matmul(plg[:], lhsT=hTf[:, k, :], rhs=rw[:, k, :], start=(k == 0), stop=(k == 7)),
                                      r=[hTf, rw], j=[plg])
                            kk.op(dve, lambda: V.tensor_tensor(out=lg[:], in0=plg[:], in1=rb[:], op=ALU.add), r=[plg, rb], w=[lg])
                            kk.op(dve, lambda: V.max(out=mx8[:], in_=lg[:]), r=[lg], w=[mx8])
                            kk.op(dve, lambda: V.tensor_scalar(out=msk[:], in0=lg[:], scalar1=mx8[:, 3:4], scalar2=None, op0=ALU.is_ge),
                                  r=[lg, mx8], w=[msk])
                            kk.op(dve, lambda: V.tensor_scalar(out=nmx[:], in0=mx8[:, 0:1], scalar1=-1.0, scalar2=None, op0=ALU.mult),
                                  r=[mx8], w=[nmx])
                            kk.op(act, lambda: A.activation(out=ee[:], in_=lg[:], func=AF.Exp, bias=nmx[:, 0:1]), r=[lg, nmx], w=[ee])
                            kk.op(dve, lambda: V.tensor_tensor(out=ee[:], in0=ee[:], in1=msk[:], op=ALU.mult), r=[ee, msk], w=[ee])
                            kk.op(dve, lambda: V.tensor_reduce(out=dn[:], in_=ee[:], axis=AX.X, op=ALU.add), r=[ee], w=[dn])
                            kk.op(dve, lambda: V.reciprocal(out=dn[:], in_=dn[:]), r=[dn], w=[dn])
                            kk.op(dve, lambda: V.tensor_scalar(out=comb[:, j, :], in0=ee[:], scalar1=dn[:, 0:1], scalar2=None, op0=ALU.mult),
                                  r=[ee, dn], j=[comb])
                            kk.op(dve, lambda: V.tensor_scalar(out=msk[:], in0=ee[:], scalar1=dn[:, 0:1], scalar2=None, op0=ALU.mult),
                                  r=[ee, dn], w=[msk])
                            kk.op(pe, lambda: PE.transpose(out=pct[:], in_=msk[:], identity=identf[:]), r=[msk, identf], w=[pct])
                            kk.op(dve, lambda: V.tensor_copy(out=cT[:], in_=pct[:]), r=[pct], w=[cT])
                            for n in range(2):
                                kk.op(pe, lambda: PE.matmul(pbd[n][:], lhsT=cT[:], rhs=bd[:, n * 512:(n + 1) * 512], start=True, stop=True),
                                      r=[cT, bd], w=[pbd[n]])
                                kk.op(dve, lambda: V.tensor_copy(out=accs[j][:, n * 512:(n + 1) * 512], in_=pbd[n][:]), r=[pbd[n]], j=[accs[j]])
                        kk.barrier()
                    if debug and b == 0 and l == 0:
                        kk.dma(sp, dbg_comb[:, :, :], comb[:], r=[comb])
                    with contextlib.ExitStack() as st2:
                        NWB = 4
                        wbuf = [sb(st2, "wexp%d" % i, [128, 8, D], BF16) for i in range(NWB)]
                        actT = [sb(st2, "actT%d" % i, [128, 8, TG], BF16) for i in range(2)]
                        gt = [sb(st2, "gt%d" % i, [128, TG]) for i in range(2)]
                        sg = [sb(st2, "sg%d" % i, [128, TG]) for i in range(2)]
                        lt = [sb(st2, "lt%d" % i, [128, TG]) for i in range(2)]
                        pg = [ps(st2, "pg%d" % i, [128, TG]) for i in range(2)]
                        pu = [ps(st2, "pu%d" % i, [128, TG]) for i in range(2)]
                        pd = [ps(st2, "pd%d" % i, [128, 512]) for i in range(3)]
                        wsrc = [w_gate, w_up, w_down]
                        wi = [0]
                        loaded = {}

                        def load_w(e, which):
                            wb_ = wbuf[wi[0] % NWB]
                            wi[0] += 1
                            kk.dma(pool, wb_[:], wsrc[which][l, e, :, :].rearrange("(k p) n -> p k n", p=128), w=[wb_])
                            loaded[(e, which)] = wb_

                        for which in range(3):
                            load_w(0, which)
                        mc = 0
                        dcl = [0]
                        pending = [None]
                        for e in range(NE):
                            wg_, wu_, wd_ = loaded[(e, 0)], loaded[(e, 1)], loaded[(e, 2)]
                            for tg in range(NTG):
                                tl = tg * TG
                                a_T = actT[(e * NTG + tg) % 2]
                                for m in range(8):
                                    p_g = pg[mc % 2]
                                    p_u = pu[mc % 2]
                                    g_t = gt[mc % 2]
                                    s_g = sg[mc % 2]
                                    l_t = lt[mc % 2]
                                    mc += 1
                                    for k in range(8):
                                        kk.op(pe, lambda: PE.matmul(p_g[:], lhsT=wg_[:, k, m * 128:(m + 1) * 128], rhs=h2T[:, k, tl:tl + TG],
                                                                    start=(k == 0), stop=(k == 7)), r=[wg_, h2T], j=[p_g])
                                    for k in range(8):
                                        kk.op(pe, lambda: PE.matmul(p_u[:], lhsT=wu_[:, k, m * 128:(m + 1) * 128], rhs=h2T[:, k, tl:tl + TG],
                                                                    start=(k == 0), stop=(k == 7)), r=[wu_, h2T], j=[p_u])
                                    kk.op(dve, lambda: V.tensor_scalar(out=g_t[:], in0=p_g[:], scalar1=bg[:, e, m:m + 1], scalar2=7.0,
                                                                       op0=ALU.add, op1=ALU.min), r=[p_g, bg], w=[g_t])
                                    kk.op(act, lambda: A.activation(out=s_g[:], in_=g_t[:], func=AF.Sigmoid, scale=1.702), r=[g_t], w=[s_g])
                                    kk.op(dve, lambda: V.tensor_scalar(out=l_t[:], in0=p_u[:], scalar1=bu[:, e, m:m + 1], scalar2=8.0,
                                                                       op0=ALU.add, op1=ALU.min), r=[p_u, bu], w=[l_t])
                                    kk.op(dve, lambda: V.tensor_tensor(out=g_t[:], in0=g_t[:], in1=s_g[:], op=ALU.mult), r=[g_t, s_g], w=[g_t])
                                    kk.op(dve, lambda: V.scalar_tensor_tensor(out=a_T[:, m, :], in0=l_t[:], scalar=-6.0, in1=g_t[:],
                                                                              op0=ALU.max, op1=ALU.mult),
                                          r=[g_t, l_t], j=[a_T])
                                if pending[0] is not None:
                                    pending[0]()
                                if tg == 0 and e + 1 < NE:
                                    load_w(e + 1, 0)
                                if tg == NTG - 1 and e + 1 < NE:
                                    load_w(e + 1, 1)
                                    load_w(e + 1, 2)
                                def mk_down(a_T=a_T, wd_=wd_, tg=tg, e=e):
                                    def emit():
                                        for i in range(TG // 128):
                                            jt = tg * (TG // 128) + i
                                            for n in range(2):
                                                p_d = pd[dcl[0] % 3]
                                                dcl[0] += 1
                                                for m in range(8):
                                                    kk.op(pe, lambda: PE.matmul(p_d[:], lhsT=a_T[:, m, i * 128:(i + 1) * 128],
                                                                                rhs=wd_[:, m, n * 512:(n + 1) * 512], start=(m == 0), stop=(m == 7)),
                                                          r=[a_T, wd_], j=[p_d])
                                                kk.op(dve, lambda: V.scalar_tensor_tensor(out=accs[jt][:, n * 512:(n + 1) * 512], in0=p_d[:],
                                                                                          scalar=comb[:, jt, e:e + 1],
                                                                                          in1=accs[jt][:, n * 512:(n + 1) * 512],
                                                                                          op0=ALU.mult, op1=ALU.add),
                                                      r=[p_d, comb, accs[jt]], w=[accs[jt]])
                                    return emit
                                pending[0] = mk_down()
                        if pending[0] is not None:
                            pending[0]()
                            pending[0] = None
                        kk.barrier()
                    with contextlib.ExitStack() as st3:
                        g2 = sb(st3, "g2bc", [128, D])
                        bc_load(sp, g2, modd[l, b:b + 1, 5 * D:6 * D])
                        xt = [sb(st3, "fxt%d" % i, [128, D]) for i in range(2)]
                        xn = [sb(st3, "fxn%d" % i, [128, D]) for i in range(2)]
                        for j in range(NTS):
                            r0 = b * S + j * 128
                            x_t = xt[j % 2]
                            x_n = xn[j % 2]
                            kk.dma(sp, x_t[:], xres[r0:r0 + 128, :], w=[x_t])
                            kk.op(dve, lambda: V.tensor_tensor(out=x_n[:], in0=accs[j][:], in1=g2[:], op=ALU.mult), r=[accs[j], g2], w=[x_n])
                            kk.op(pool, lambda: G.tensor_tensor(out=x_n[:], in0=x_n[:], in1=x_t[:], op=ALU.add), r=[x_n, x_t], w=[x_n])
                            kk.dma(sp, dst[r0:r0 + 128, :], x_n[:], r=[x_n])
                        kk.barrier()

        def phase_mixer(l):
            for b in range(NB):
                with contextlib.ExitStack() as st:
                    aoT = sb(st, "aoT", [128, 8, S], BF16)
                    phase_attn(l, b, aoT)
                    ysT = sb(st, "ysT", [128, 16, S], BF16)
                    if debug and b == 0 and l == 0:
                        kk.dma(sp, dbg_aoT[:, :, :], aoT[:], r=[aoT])
                    if stop == "attn":
                        kk.barrier()
                        continue
                    phase_ssd(l, b, ysT)
                    if debug and b == 0 and l == 0 and stop not in ("ssd_pre", "ssd_nofin"):
                        kk.dma(sp, dbg_ysT[:, :, :], ysT[:], r=[ysT])
                    if stop in ("ssd", "ssd_pre", "ssd_nofin"):
                        kk.barrier()
                        continue
                    phase_outproj(l, b, aoT, ysT)

        oneb = sb(es, "oneb", [128, 1])
        kk.op(pool, lambda: G.memset(oneb[:], 1.0), w=[oneb])

        for l in range(nlayers):
            phase_mod(l)
            if stop == "mod":
                break
            phase_inproj(l)
            if stop == "inproj":
                break
            phase_mixer(l)
            if stop in ("mixer", "attn", "ssd", "ssd_pre", "ssd_nofin"):
                break
            phase_moe(l, final=(l == nlayers - 1))
            if stop == "moe":
                break
        kk.barrier()
        print("instructions:", kk.ninstr)
    return nc


def host_inputs(inputs, NB, S, ncores, ne_decl=NE):
    f = lambda a: np.ascontiguousarray(np.asarray(a))
    x = f(inputs["x"]).astype(np.float32, copy=False)
    c = f(inputs["c"])
    pos = f(inputs["positions"]).astype(np.int32, copy=False)
    Lh = inputs["ada_w"].shape[0]
    shared = {
        "ada_w": f(inputs["ada_w"]), "ada_b": f(inputs["ada_b"]),
        "norm1_g": f(inputs["norm1_g"]), "norm2_g": f(inputs["norm2_g"]),
        "w_in": f(inputs["w_in"]), "q_norm_g": f(inputs["q_norm_g"]), "k_norm_g": f(inputs["k_norm_g"]),
        "attn_sink": f(inputs["attn_sink"]),
        "conv_w": f(np.asarray(inputs["conv_w"]).reshape(Lh, 5, 24, 128).transpose(0, 3, 2, 1)),
        "conv_b": f(np.asarray(inputs["conv_b"]).reshape(Lh, 24, 128).transpose(0, 2, 1)),
        "a_log": f(np.asarray(inputs["a_log"]).reshape(Lh, 64)),
        "dt_bias": f(np.asarray(inputs["dt_bias"]).reshape(Lh, 64)),
        "ssm_d": f(inputs["ssm_d"]), "ssm_norm_g": f(inputs["ssm_norm_g"]),
        "w_attn_o": f(inputs["w_attn_o"]), "w_ssm_o": f(inputs["w_ssm_o"]), "w_out": f(inputs["w_out"]),
        "router_w": f(inputs["router_w"]), "router_b": f(inputs["router_b"]),
        "exp_w_gate": f(np.asarray(inputs["exp_w_gate"])[:, :ne_decl]), "exp_w_up": f(np.asarray(inputs["exp_w_up"])[:, :ne_decl]), "exp_w_down": f(np.asarray(inputs["exp_w_down"])[:, :ne_decl]),
        "exp_b_gate": f(np.asarray(inputs["exp_b_gate"]).reshape(Lh, NE, 8, 128).transpose(0, 3, 1, 2)),
        "exp_b_up": f(np.asarray(inputs["exp_b_up"]).reshape(Lh, NE, 8, 128).transpose(0, 3, 1, 2)),
        "exp_b_down": f(inputs["exp_b_down"]),
    }
    invf = (np.float32(500000.0) ** (-np.arange(0, 16, 2, dtype=np.float32) / np.float32(16))).astype(np.float32)
    shared["invf"] = f(np.broadcast_to(invf[None, :], (128, 8)))
    maps = []
    for i in range(ncores):
        bs = slice(i * NB, (i + 1) * NB)
        m = dict(shared)
        m["x"] = f(x[bs].reshape(NB * S, D))
        m["cT"] = f(c[bs].reshape(NB, 8, 128).transpose(2, 1, 0))
        m["pos"] = f(pos[bs].reshape(NB * S // 128, 128).T)
        maps.append(m)
    return maps


_NC_CACHE = {}


def kernel(**inputs):
    B, S, _ = inputs["x"].shape
    ncores = 8
    NB = B // ncores
    key = (NB, S)
    if key not in _NC_CACHE:
        _NC_CACHE[key] = build(NB, S)
    nc = _NC_CACHE[key]
    maps = host_inputs(inputs, NB, S, ncores)
    res = run_bass_kernel_spmd(nc, maps, core_ids=list(range(ncores)))
    out = np.concatenate([np.asarray(r["y"]).reshape(NB, S, D) for r in res.results], axis=0)
    return out.astype(np.float32, copy=False)
```
